# Optimizing a Trainium2 kernel written in Bass

```python
import math
import jax, jax.numpy as jnp
from jax import lax
import numpy as np

D_MODEL = 1024
BATCH = 16
SEQ = 2048
DEPTH = 2

CTX_LEN = 256
GRID_W = 64
N_GROUPS = 4
GROUP_W = D_MODEL // N_GROUPS
N_SPLITS = 10
D_IN = N_SPLITS * GROUP_W
N_HEADS_A = 4
HEAD_A = GROUP_W // N_HEADS_A
CHUNK = 128
N_HEADS_B = 4
HEAD_V_B = GROUP_W // N_HEADS_B
HEAD_QK_B = HEAD_V_B // 2
Q_BLOCK = 128
ROPE_BASE = 10000.0
CONV_C = 31
CONV_D = 3
N_EXPERTS = 32
TOP_K = 4
D_FF = D_MODEL
SWIGLU_LIMIT = 7.0
SWIGLU_ALPHA = 1.702
EPS = 1e-6

kernel_name = "hybrid_headgroup_diffusion_moe"


def rms_norm(x, g):
    xf = x.astype(jnp.float32)
    y = xf * lax.rsqrt(jnp.mean(xf * xf, axis=-1, keepdims=True) + EPS)
    return (y * g.astype(jnp.float32)).astype(x.dtype)


def layer_norm(x, g, b):
    xf = x.astype(jnp.float32)
    mu = jnp.mean(xf, axis=-1, keepdims=True)
    var = jnp.mean(jnp.square(xf - mu), axis=-1, keepdims=True)
    y = (xf - mu) * lax.rsqrt(var + EPS) * g.astype(jnp.float32) + b.astype(jnp.float32)
    return y.astype(x.dtype)


def axial_rope_tables(n):
    rows = n // GRID_W
    row = jnp.repeat(jnp.arange(rows, dtype=jnp.float32), GRID_W)
    col = jnp.tile(jnp.arange(GRID_W, dtype=jnp.float32), rows)
    nf = HEAD_QK_B // 4
    inv = ROPE_BASE ** (-jnp.arange(nf, dtype=jnp.float32) / nf)
    ar = row[:, None] * inv
    ac = col[:, None] * inv
    return jnp.cos(ar), jnp.sin(ar), jnp.cos(ac), jnp.sin(ac)


def _rotate(x, cos, sin):
    h = x.shape[-1] // 2
    x1, x2 = x[..., :h], x[..., h:]
    return jnp.concatenate([x1 * cos - x2 * sin, x1 * sin + x2 * cos], axis=-1)


def apply_axial_rope(x, tables):
    cr, sr, cc, sc = [t[None, :, None, None, :] for t in tables]
    xf = x.astype(jnp.float32)
    half = x.shape[-1] // 2
    out = jnp.concatenate([_rotate(xf[..., :half], cr, sr), _rotate(xf[..., half:], cc, sc)], axis=-1)
    return out.astype(x.dtype)


def dwconv(x, w):
    k = w.shape[0]
    return lax.conv_general_dilated(x, w[:, None, :], (1,), [(k // 2, k // 2)],
                                    dimension_numbers=("NWC", "WIO", "NWC"),
                                    feature_group_count=x.shape[-1])


def chunk_gmlp(au, av, vnorm_g, w_s, b_s):
    bsz, n, _ = au.shape
    u = jax.nn.gelu(au)
    v = jax.nn.gelu(av).reshape(bsz, n // CHUNK, CHUNK, N_HEADS_A, HEAD_A)
    v = rms_norm(v, vnorm_g.reshape(N_HEADS_A, HEAD_A))
    mixed = jnp.einsum("hpq,bnqhc->bnphc", w_s, v) + b_s.T[:, :, None]
    return u * mixed.reshape(bsz, n, GROUP_W)


def qk_heads(t, g):
    bsz, n, _ = t.shape
    return rms_norm(t.reshape(bsz, n, N_HEADS_B, 2, HEAD_QK_B), g)


def v_heads(t):
    bsz, n, _ = t.shape
    return t.reshape(bsz, n, N_HEADS_B, HEAD_V_B)


def diff_attention(q, k, v, lam):
    s = jnp.einsum("bqhmd,bkhmd->bhmqk", q, k).astype(jnp.float32) * (q.shape[-1] ** -0.5)
    p = jax.nn.softmax(s, axis=-1)
    w = p[:, :, 0] - lam * p[:, :, 1]
    return jnp.einsum("bhqk,bkhe->bqhe", w.astype(v.dtype), v)


def diff_attention_blocks(q, k, v, lam):
    bsz, n = q.shape[:2]
    qb = q.reshape(bsz, n // Q_BLOCK, Q_BLOCK, *q.shape[2:]).swapaxes(0, 1)
    out = lax.map(lambda qq: diff_attention(qq, k, v, lam), qb)
    return out.swapaxes(0, 1).reshape(bsz, n, N_HEADS_B, HEAD_V_B)


def diff_post(y, g, lam_init):
    bsz, n = y.shape[:2]
    return (rms_norm(y, g) * (1.0 - lam_init)).reshape(bsz, n, GROUP_W)


def conformer_conv(ca, cg, w, b, ln_g, ln_b):
    z = ca * jax.nn.sigmoid(cg)
    z = dwconv(z, w) + b
    return jax.nn.silu(layer_norm(z, ln_g, ln_b))


def short_gated_conv(db, dc, dh, w):
    return db * dwconv(dc * dh, w)


def moe_ffn(t, w_r, b_r, w_gu, b_gu, w_dn, b_dn):
    logits = (t @ w_r + b_r).astype(jnp.float32)
    top_v, top_i = lax.top_k(logits, TOP_K)
    gates = jnp.einsum("nk,nke->ne", jax.nn.softmax(top_v, axis=-1),
                       jax.nn.one_hot(top_i, N_EXPERTS, dtype=jnp.float32))

    def expert(acc, xs):
        wgu, bgu, wdn, bdn, g = xs
        gl, up = jnp.split(t @ wgu + bgu, 2, axis=-1)
        gl = jnp.minimum(gl, SWIGLU_LIMIT)
        up = jnp.clip(up, -SWIGLU_LIMIT, SWIGLU_LIMIT)
        hid = (up + 1.0) * gl * jax.nn.sigmoid(SWIGLU_ALPHA * gl)
        return acc + g[:, None].astype(t.dtype) * (hid @ wdn + bdn), None

    out, _ = lax.scan(expert, jnp.zeros_like(t), (w_gu, b_gu, w_dn, b_dn, gates.T))
    return out


def setup_inputs(seed: int = 0) -> dict:
    key = jax.random.key(seed)
    ks = iter(jax.random.split(key, 40))

    def nrm(shape, scale):
        return jax.random.normal(next(ks), shape, jnp.float32) * scale

    L, D, G, E, F = DEPTH, D_MODEL, GROUP_W, N_EXPERTS, D_FF
    return {
        "x": nrm((BATCH, SEQ, D), 1.0),
        "c": nrm((BATCH, D), 1.0),
        "ctx": nrm((BATCH, CTX_LEN, D), 1.0),
        "c_ctx": nrm((D,), 1.0),
        "w_mod": nrm((L, D, 6 * D), 0.5 * D ** -0.5),
        "b_mod": nrm((L, 6 * D), 0.02),
        "norm1_g": 1.0 + nrm((L, D), 0.02),
        "norm2_g": 1.0 + nrm((L, D), 0.02),
        "w_in": nrm((L, D, D_IN), D ** -0.5),
        "a_vnorm_g": 1.0 + nrm((L, G), 0.02),
        "a_ws": nrm((L, N_HEADS_A, CHUNK, CHUNK), CHUNK ** -0.5),
        "a_bs": 1.0 + nrm((L, N_HEADS_A, CHUNK), 0.1),
        "b_qnorm_g": 1.0 + nrm((L, 2, HEAD_QK_B), 0.02),
        "b_knorm_g": 1.0 + nrm((L, 2, HEAD_QK_B), 0.02),
        "b_lam_q1": nrm((L, HEAD_QK_B), 0.1),
        "b_lam_k1": nrm((L, HEAD_QK_B), 0.1),
        "b_lam_q2": nrm((L, HEAD_QK_B), 0.1),
        "b_lam_k2": nrm((L, HEAD_QK_B), 0.1),
        "b_subln_g": 1.0 + nrm((L, HEAD_V_B), 0.02),
        "c_conv_w": nrm((L, CONV_C, G), CONV_C ** -0.5),
        "c_conv_b": nrm((L, G), 0.02),
        "c_ln_g": 1.0 + nrm((L, G), 0.02),
        "c_ln_b": nrm((L, G), 0.02),
        "d_conv_w": nrm((L, CONV_D, G), CONV_D ** -0.5),
        "w_out": nrm((L, D, D), D ** -0.5),
        "router_w": nrm((L, D, E), D ** -0.5),
        "router_b": nrm((L, E), 0.01),
        "exp_w_gu": nrm((L, E, D, 2 * F), D ** -0.5),
        "exp_b_gu": nrm((L, E, 2 * F), 0.01),
        "exp_w_dn": nrm((L, E, F, D), F ** -0.5),
        "exp_b_dn": nrm((L, E, D), 0.01),
    }


def reference(x, c, ctx, c_ctx, w_mod, b_mod, norm1_g, norm2_g, w_in, a_vnorm_g, a_ws, a_bs,
              b_qnorm_g, b_knorm_g, b_lam_q1, b_lam_k1, b_lam_q2, b_lam_k2, b_subln_g,
              c_conv_w, c_conv_b, c_ln_g, c_ln_b, d_conv_w, w_out, router_w, router_b,
              exp_w_gu, exp_b_gu, exp_w_dn, exp_b_dn):
    bsz, seq, dm = x.shape
    rope = axial_rope_tables(seq)
    xc = ctx
    for l in range(DEPTH):
        last = l == DEPTH - 1
        lam_init = 0.8 - 0.6 * math.exp(-0.3 * l)
        lam = (jnp.exp(jnp.sum(b_lam_q1[l] * b_lam_k1[l]).astype(jnp.float32))
               - jnp.exp(jnp.sum(b_lam_q2[l] * b_lam_k2[l]).astype(jnp.float32)) + lam_init)
        sh1, sc1, g1, sh2, sc2, g2 = jnp.split((jax.nn.silu(c) @ w_mod[l] + b_mod[l])[:, None, :], 6, axis=-1)
        sh1c, sc1c, g1c, sh2c, sc2c, g2c = jnp.split(jax.nn.silu(c_ctx) @ w_mod[l] + b_mod[l], 6)
        h = rms_norm(x, norm1_g[l]) * (1.0 + sc1) + sh1
        hc = rms_norm(xc, norm1_g[l]) * (1.0 + sc1c) + sh1c
        au, av, bq, bk, bv, ca, cg, db, dc, dh = jnp.split(h @ w_in[l], N_SPLITS, axis=-1)
        if last:
            bkc = hc @ w_in[l][:, 3 * GROUP_W:4 * GROUP_W]
            bvc = hc @ w_in[l][:, 4 * GROUP_W:5 * GROUP_W]
        else:
            auc, avc, bqc, bkc, bvc, cac, cgc, dbc, dcc, dhc = jnp.split(hc @ w_in[l], N_SPLITS, axis=-1)
        kc = qk_heads(bkc, b_knorm_g[l])
        vc = v_heads(bvc)
        q = apply_axial_rope(qk_heads(bq, b_qnorm_g[l]), rope)
        k = apply_axial_rope(qk_heads(bk, b_knorm_g[l]), rope)
        yb = diff_attention_blocks(q, jnp.concatenate([k, kc], axis=1),
                                   jnp.concatenate([v_heads(bv), vc], axis=1), lam)
        y = jnp.concatenate([
            chunk_gmlp(au, av, a_vnorm_g[l], a_ws[l], a_bs[l]),
            diff_post(yb, b_subln_g[l], lam_init),
            conformer_conv(ca, cg, c_conv_w[l], c_conv_b[l], c_ln_g[l], c_ln_b[l]),
            short_gated_conv(db, dc, dh, d_conv_w[l]),
        ], axis=-1)
        x = x + g1 * (y @ w_out[l])
        h2 = rms_norm(x, norm2_g[l]) * (1.0 + sc2) + sh2
        moe_args = (router_w[l], router_b[l], exp_w_gu[l], exp_b_gu[l], exp_w_dn[l], exp_b_dn[l])
        if last:
            x = x + g2 * moe_ffn(h2.reshape(-1, dm), *moe_args).reshape(x.shape)
        else:
            ycb = diff_attention(qk_heads(bqc, b_qnorm_g[l]), kc, vc, lam)
            yc = jnp.concatenate([
                chunk_gmlp(auc, avc, a_vnorm_g[l], a_ws[l], a_bs[l]),
                diff_post(ycb, b_subln_g[l], lam_init),
                conformer_conv(cac, cgc, c_conv_w[l], c_conv_b[l], c_ln_g[l], c_ln_b[l]),
                short_gated_conv(dbc, dcc, dhc, d_conv_w[l]),
            ], axis=-1)
            xc = xc + g1c * (yc @ w_out[l])
            h2c = rms_norm(xc, norm2_g[l]) * (1.0 + sc2c) + sh2c
            ff = moe_ffn(jnp.concatenate([h2.reshape(-1, dm), h2c.reshape(-1, dm)], axis=0), *moe_args)
            x = x + g2 * ff[:bsz * seq].reshape(x.shape)
            xc = xc + g2c * ff[bsz * seq:].reshape(xc.shape)
    return x
```

```python
import math
from contextlib import ExitStack
import numpy as np
import concourse.bass as bass
import concourse.mybir as mybir
from concourse.bass_utils import run_bass_kernel_spmd
from concourse.bass import IndirectOffsetOnAxis

F32 = mybir.dt.float32
BF16 = mybir.dt.bfloat16
ALU = mybir.AluOpType
AF = mybir.ActivationFunctionType

NCORES = 8
DEPTH = 2
D = 1024
SEQ = 2048
CTX = 256
NB = 2
EPS = 1e-6
NEXP = 32
MOE_SPARSE = True
U32 = mybir.dt.uint32
ENGS = ("pe", "act", "dve", "pool", "sp")


class T:
    __slots__ = ("name", "w", "r")

    def __init__(self, name=""):
        self.name = name
        self.w = None
        self.r = []


class Prog:
    def __init__(self, nc, stack, n_dma_sems=8):
        self.nc = nc
        self.q = {e: [] for e in ENGS}
        self.cnt = {e: 0 for e in ENGS}
        self.sem = {e: stack.enter_context(nc.semaphore("s_" + e)) for e in ENGS}
        self.seen = {e: {} for e in ENGS}
        self.dsem, self.dval, self.dnext = {}, {}, {}
        for qn in ("sp", "act", "pool"):
            self.dsem[qn] = [stack.enter_context(nc.semaphore(f"d_{qn}{i}")) for i in range(n_dma_sems)]
            self.dval[qn] = [0] * n_dma_sems
            self.dnext[qn] = 0
        self.pending = {e: False for e in ENGS}

    def _need(self, eng, ev, waits):
        if ev is None:
            return
        key = ev[0:2]
        if eng == "pe" and key == ("eng", "pe"):
            return
        if self.seen[eng].get(key, 0) >= ev[2]:
            return
        if waits.get(key, 0) < ev[2]:
            waits[key] = ev[2]

    def _deps(self, eng, reads, writes):
        waits = {}
        for t in reads:
            self._need(eng, t.w, waits)
        for t in writes:
            self._need(eng, t.w, waits)
            for ev in t.r:
                self._need(eng, ev, waits)
        for key, v in waits.items():
            self.seen[eng][key] = v
        return list(waits.items())

    def _mark(self, ev, reads, writes):
        for t in reads:
            t.r.append(ev)
            if len(t.r) > 48:
                best = {}
                for e in t.r:
                    k = e[0:2]
                    if best.get(k, 0) < e[2]:
                        best[k] = e[2]
                t.r = [(k[0], k[1], v) for k, v in best.items()]
        for t in writes:
            t.w = ev
            t.r = []

    def op(self, eng, fn, reads=(), writes=(), inc=True):
        waits = self._deps(eng, reads, writes)
        ev = ("eng", eng, self.cnt[eng] + 1)
        if inc:
            self.cnt[eng] += 1
            self.pending[eng] = False
        else:
            self.pending[eng] = True
        self.q[eng].append((waits, fn, inc, None))
        self._mark(ev, reads, writes)

    def dma(self, qn, fn, reads=(), writes=()):
        waits = self._deps(qn, reads, writes)
        i = self.dnext[qn]
        self.dnext[qn] = (i + 1) % len(self.dsem[qn])
        if self.dval[qn][i] > 0:
            key = ("dma", (qn, i))
            if self.seen[qn].get(key, 0) < self.dval[qn][i]:
                waits.append((key, self.dval[qn][i]))
                self.seen[qn][key] = self.dval[qn][i]
        self.dval[qn][i] += 16
        ev = ("dma", (qn, i), self.dval[qn][i])
        self.q[qn].append((waits, fn, False, (qn, i)))
        self._mark(ev, reads, writes)

    def barrier(self):
        evs = [("eng", e, self.cnt[e]) for e in ENGS if self.cnt[e] > 0]
        for qn in self.dsem:
            for i, v in enumerate(self.dval[qn]):
                if v > 0:
                    evs.append(("dma", (qn, i), v))
        for e in ENGS:
            assert not self.pending[e]
            waits = {}
            for ev in evs:
                self._need(e, ev, waits)
            for key, v in waits.items():
                self.seen[e][key] = v
            self.q[e].append((list(waits.items()), None, False, None))

    def check(self):
        val = {}
        pos = {e: 0 for e in ENGS}
        progress = True
        while progress:
            progress = False
            for e in ENGS:
                q = self.q[e]
                while pos[e] < len(q):
                    waits, fn, inc, d = q[pos[e]]
                    if any(val.get(k, 0) < v for k, v in waits):
                        break
                    if fn is not None:
                        if d is not None:
                            val[("dma", d)] = val.get(("dma", d), 0) + 16
                        elif inc:
                            val[("eng", e)] = val.get(("eng", e), 0) + 1
                    pos[e] += 1
                    progress = True
        stuck = {e: (pos[e], len(self.q[e])) for e in ENGS if pos[e] < len(self.q[e])}
        if stuck:
            msg = []
            for e, (p, n) in stuck.items():
                waits = self.q[e][p][0]
                msg.append(f"{e} stuck at {p}/{n} waits={[(k, v, val.get(k, 0)) for k, v in waits if val.get(k, 0) < v]}")
            raise RuntimeError("deadlock: " + "; ".join(msg))

    def _semof(self, key):
        if key[0] == "eng":
            return self.sem[key[1]]
        qn, i = key[1]
        return self.dsem[qn][i]

    def emit(self):
        nc = self.nc
        for e in ENGS:
            assert not self.pending[e], e
        self.check()
        with nc.Block() as block:
            def run(engname):
                def body(eng):
                    for waits, fn, inc, d in self.q[engname]:
                        for key, v in waits:
                            eng.wait_ge(self._semof(key), v)
                        if fn is None:
                            continue
                        ins = fn(eng)
                        if d is not None:
                            ins.then_inc(self.dsem[d[0]][d[1]], 16)
                        elif inc:
                            ins.then_inc(self.sem[engname], 1)
                return body
            block.tensor(run("pe"))
            block.scalar(run("act"))
            block.vector(run("dve"))
            block.gpsimd(run("pool"))
            block.sync(run("sp"))


PARAM_SHAPES = {
    "w_mod": [DEPTH, D, 6 * D], "b_mod": [DEPTH, 6 * D], "norm1_g": [DEPTH, D], "norm2_g": [DEPTH, D],
    "w_in": [DEPTH, D, 2560], "a_vnorm_g": [DEPTH, 256], "a_ws": [DEPTH, 4, 128, 128], "a_bs": [DEPTH, 512],
    "b_qnorm_g": [DEPTH, 64], "b_knorm_g": [DEPTH, 64], "b_lam_q1": [DEPTH, 32], "b_lam_k1": [DEPTH, 32],
    "b_lam_q2": [DEPTH, 32], "b_lam_k2": [DEPTH, 32], "b_subln_g": [DEPTH, 64],
    "c_conv_w": [DEPTH, 31, 256], "c_conv_b": [DEPTH, 256], "c_ln_g": [DEPTH, 256], "c_ln_b": [DEPTH, 256],
    "d_conv_w": [DEPTH, 3, 256], "w_out": [DEPTH, D, D], "router_w": [DEPTH, D, NEXP], "router_b": [DEPTH, NEXP],
    "exp_w_gu": [DEPTH, NEXP, D, 2 * D], "exp_b_gu": [DEPTH, NEXP, 2 * D], "exp_w_dn": [DEPTH, NEXP, D, D],
    "exp_b_dn": [DEPTH, NEXP, D],
}


class _Stop(Exception):
    pass


def build_nc(n_layers=DEPTH, dbg=None, skip_moe=False, stop=None):
    nc = bass.Bass("TRN2", target_bir_lowering=False)
    din = lambda n, s: nc.dram_tensor(n, s, F32, kind="ExternalInput").ap()
    x_in = din("x", [NB, SEQ, D])
    ctx_in = din("ctx", [NB, CTX, D])
    c_in = din("c", [3, D])
    prm = {k: din(k, s) for k, s in PARAM_SHAPES.items()}
    ident_in = din("k_ident", [128, 128])
    blk_in = din("k_blk", [64, 64])
    perm_in = din("k_perm", [64, 64])
    ropec_in = din("k_ropec", [64, SEQ])
    ropes_in = din("k_ropes", [64, SEQ])
    ltri_in = din("k_ltri", [128, 128])
    eiota_in = din("k_eiota", [128, 1])
    pk_in = din("k_pk", [128, 16])
    out_d = nc.dram_tensor("out", [NB, SEQ, D], F32, kind="ExternalOutput").ap()
    NTOK = NB * (SEQ + CTX)
    xa_d = nc.dram_tensor("xa_scr", [NTOK, D], F32, kind="Internal").ap()
    xb_d = nc.dram_tensor("xb_scr", [NTOK, D], F32, kind="Internal").ap()
    mod_d = nc.dram_tensor("mod_scr", [DEPTH, 3, 6 * D], F32, kind="Internal").ap()
    NSLOT = (NTOK * 4 // 512 + NEXP) * 512
    h2_d = nc.dram_tensor("h2_scr", [NTOK, D], F32, kind="Internal").ap()
    xp_d = nc.dram_tensor("xp_scr", [NSLOT, D], F32, kind="Internal").ap()
    y_d = nc.dram_tensor("y_scr", [NSLOT, D], F32, kind="Internal").ap()
    wgu_flat = prm["exp_w_gu"].rearrange("l e k f -> (l e k) f")
    wdn_flat = prm["exp_w_dn"].rearrange("l e k f -> (l e k) f")
    dbg_d = {}
    if dbg:
        for name, shape in dbg.items():
            dbg_d[name] = nc.dram_tensor(name, shape, F32, kind="ExternalOutput").ap()

    seqs = []
    off = 0
    for b in range(NB):
        seqs.append(dict(ctx=True, b=b, n=CTX, off=off, mv=2)); off += CTX
        seqs.append(dict(ctx=False, b=b, n=SEQ, off=off, mv=b)); off += SEQ
    t_xa, t_xb, t_mod, t_out = T("xa"), T("xb"), T("mod"), T("out")

    def seq_src(s, l):
        if l == 0:
            return (ctx_in[s["b"]] if s["ctx"] else x_in[s["b"]]), None
        return xb_d[s["off"]:s["off"] + s["n"], :], t_xb

    with ExitStack() as st:
        P = Prog(nc, st)
        ncd = nc.allow_non_contiguous_dma(reason="small parameter layout loads")
        st.enter_context(ncd)

        def mm(out, lhsT, rhs, start, stop, r=(), w=()):
            P.op("pe", lambda e: e.matmul(out, lhsT=lhsT, rhs=rhs, start=start, stop=stop), r, w, inc=stop)

        def tr(out, in_, ident, r=(), w=()):
            P.op("pe", lambda e: e.transpose(out, in_, ident), r, w)

        def act(out, in_, func, r=(), w=(), bias=0.0, scale=1.0, accum=None):
            if accum is None:
                P.op("act", lambda e: e.activation(out, in_, func, bias=bias, scale=scale), r, w)
            else:
                P.op("act", lambda e: e.activation(out, in_, func, bias=bias, scale=scale, accum_out=accum), r, w)

        def tt(eng, out, a, b, op, r=(), w=()):
            P.op(eng, lambda e: e.tensor_tensor(out, a, b, op), r, w)

        def ts(eng, out, a, s1, s2, op0, op1=None, r=(), w=()):
            if op1 is None:
                P.op(eng, lambda e: e.tensor_scalar(out, a, s1, None, op0), r, w)
            else:
                P.op(eng, lambda e: e.tensor_scalar(out, a, s1, s2, op0, op1), r, w)

        def stt(out, in0, scalar, in1, op0, op1, r=(), w=()):
            P.op("dve", lambda e: e.scalar_tensor_tensor(out, in0, scalar, in1, op0, op1), r, w)

        def cp(eng, out, in_, r=(), w=()):
            if eng == "act":
                act(out, in_, AF.Identity, r, w)
            else:
                P.op(eng, lambda e: e.tensor_copy(out, in_), r, w)

        def rcp(out, in_, r=(), w=()):
            P.op("dve", lambda e: e.reciprocal(out, in_), r, w)

        def mset(eng, ap, val, w=()):
            P.op(eng, lambda e: e.memset(ap, val), (), w)

        def dma(qn, out, in_, r=(), w=()):
            P.dma(qn, lambda e: e.dma_start(out=out, in_=in_), r, w)

        _uid = [0]

        def sb(scope, name, shape, dt):
            _uid[0] += 1
            return scope.enter_context(nc.sbuf_tensor(f"{name}_u{_uid[0]}", shape, dt))

        ps = [st.enter_context(nc.psum_tensor(f"ps{i}", [128, 512], F32)) for i in range(8)]
        tps = [T(f"ps{i}") for i in range(8)]

        class Ring:
            def __init__(self, idxs):
                self.idxs = idxs
                self.i = 0

            def next(self):
                k = self.idxs[self.i % len(self.idxs)]
                self.i += 1
                return ps[k], tps[k]

        _bp = [0]

        def bp_hit():
            _bp[0] += 1
            return stop == f"bp{_bp[0]}"

        ident_f = sb(st, "ident_f", [128, 128], F32)
        ident_b = sb(st, "ident_b", [128, 128], BF16)
        blk64 = sb(st, "blk64", [64, 64], F32)
        perm64 = sb(st, "perm64", [64, 64], F32)
        ones256 = sb(st, "ones256", [128, 128], F32)
        t_const = T("const")
        dma("sp", ident_f[:], ident_in, w=[t_const])
        dma("sp", blk64[:], blk_in, w=[t_const])
        dma("sp", perm64[:], perm_in, w=[t_const])
        t_identb = T("identb")
        cp("dve", ident_b[:], ident_f[:], r=[t_const], w=[t_identb])
        t_ones = T("ones")
        mset("pool", ones256[:], 1.0 / 256.0, w=[t_ones])

        def rms_rstd(scope_tiles, x_ap, t_x, ncols, tagw):
            junk, t_junk, ss, t_ss = scope_tiles
            act(junk[:, 0:ncols], x_ap, AF.Square, r=[t_x], w=[t_junk, t_ss], accum=ss[:, 0:1])
            act(ss[:, 1:2], ss[:, 0:1], AF.Sqrt, r=[t_ss], w=[t_ss], bias=EPS, scale=1.0 / ncols)
            rcp(ss[:, 2:3], ss[:, 1:2], r=[t_ss], w=[t_ss])
            return ss[:, 2:3]

        def _layers():
          for l in range(n_layers):
            last = (l == DEPTH - 1)
            lam_init = 0.8 - 0.6 * math.exp(-0.3 * l)
            with ExitStack() as ls:
                n1g = sb(ls, f"n1g{l}", [128, 8], F32)
                n2g = sb(ls, f"n2g{l}", [128, 8], F32)
                modP = sb(ls, f"modP{l}", [128, 4, 3, 8], F32)
                AB = sb(ls, f"AB{l}", [128, 4, 3, 8], F32)
                t_lp = T("layerparams")
                dma("sp", n1g[:], prm["norm1_g"][l].rearrange("(k p) -> p k", p=128), w=[t_lp])
                dma("sp", n2g[:], prm["norm2_g"][l].rearrange("(k p) -> p k", p=128), w=[t_lp])

                with ExitStack() as ms:
                    cT = sb(ms, f"cT{l}", [128, 8, 3], F32)
                    scT = sb(ms, f"scT{l}", [128, 8, 3], BF16)
                    bmod = sb(ms, f"bmod{l}", [3, 6 * D], F32)
                    modrow = sb(ms, f"modrow{l}", [3, 6 * D], F32)
                    wms = [sb(ms, f"wms{l}_{i}", [128, 8, 512], BF16) for i in range(2)]
                    t_wms = [T("wms0"), T("wms1")]
                    t_c, t_bm, t_mr = T("c"), T("bmod"), T("modrow")
                    for v in range(3):
                        dma("sp", cT[:, :, v], c_in[v].rearrange("(k p) -> p k", p=128), w=[t_c])
                    dma("sp", bmod[:], prm["b_mod"][l:l + 1, :].partition_broadcast(3), w=[t_bm])
                    act(scT[:], cT[:], AF.Silu, r=[t_c], w=[t_c])
                    for n in range(12):
                        wb, twb = wms[n % 2], t_wms[n % 2]
                        dma("pool", wb[:], prm["w_mod"][l][:, n * 512:(n + 1) * 512].rearrange("(k p) f -> p k f", p=128), w=[twb])
                        pb, tpb = ps[n % 2], tps[n % 2]
                        for k in range(8):
                            mm(pb[0:3, :], scT[:, k, :], wb[:, k, :], k == 0, k == 7, r=[t_c, twb], w=[tpb])
                        tt("dve", modrow[:, n * 512:(n + 1) * 512], pb[0:3, :], bmod[:, n * 512:(n + 1) * 512], ALU.add,
                           r=[tpb, t_bm], w=[t_mr])
                    dma("sp", mod_d[l], modrow[:], r=[t_mr], w=[t_mod])
                    for si, sec in enumerate((0, 1, 3, 4)):
                        for v in range(3):
                            dma("sp", modP[:, si, v, :], mod_d[l, v, sec * D:(sec + 1) * D].rearrange("(k p) -> p k", p=128),
                                r=[t_mod], w=[t_lp])
                    for v in range(3):
                        stt(AB[:, 0, v, :], modP[:, 1, v, :], 1.0, n1g[:], ALU.add, ALU.mult, r=[t_lp], w=[t_lp])
                        cp("dve", AB[:, 1, v, :], modP[:, 0, v, :], r=[t_lp], w=[t_lp])
                        stt(AB[:, 2, v, :], modP[:, 3, v, :], 1.0, n2g[:], ALU.add, ALU.mult, r=[t_lp], w=[t_lp])
                        cp("dve", AB[:, 3, v, :], modP[:, 2, v, :], r=[t_lp], w=[t_lp])
                    P.barrier()
                if stop == "mod":
                    dma("sp", dbg_d["dbg_mod"], mod_d[0], r=[t_mod], w=[T("dbg2")])
                    return

                with ExitStack() as mx:
                    vngb = sb(mx, f"vngb{l}", [128, 256], F32)
                    bsb = sb(mx, f"bsb{l}", [128, 512], F32)
                    wsT = sb(mx, f"wsT{l}", [128, 4, 128], BF16)
                    gqk = sb(mx, f"gqk{l}", [64, 2], F32)
                    lamv = sb(mx, f"lamv{l}", [128, 4, 32], F32)
                    lams = sb(mx, f"lams{l}", [128, 8], F32)
                    sublnb = sb(mx, f"sublnb{l}", [128, 64], F32)
                    prow = sb(mx, f"prow{l}", [37, 256], F32)
                    pcol = sb(mx, f"pcol{l}", [128, 2, 37], F32)
                    diag = sb(mx, f"diag{l}", [128, 2, 31, 128], BF16)
                    ropec = sb(mx, f"ropec{l}", [64, SEQ], F32)
                    ropes = sb(mx, f"ropes{l}", [64, SEQ], F32)
                    t_mp = T("mixparams")
                    t_diag = T("diag")
                    dma("sp", ropec[:], ropec_in, w=[t_mp])
                    dma("sp", ropes[:], ropes_in, w=[t_mp])
                    dma("sp", vngb[:], prm["a_vnorm_g"][l:l + 1, :].partition_broadcast(128), w=[t_mp])
                    dma("sp", bsb[:], prm["a_bs"][l:l + 1, :].partition_broadcast(128), w=[t_mp])
                    dma("sp", gqk[:, 0:1], prm["b_qnorm_g"][l].rearrange("(p o) -> p o", o=1), w=[t_mp])
                    dma("sp", gqk[:, 1:2], prm["b_knorm_g"][l].rearrange("(p o) -> p o", o=1), w=[t_mp])
                    for i, nm in enumerate(("b_lam_q1", "b_lam_k1", "b_lam_q2", "b_lam_k2")):
                        dma("sp", lamv[:, i, :], prm[nm][l:l + 1, :].partition_broadcast(128), w=[t_mp])
                    dma("sp", sublnb[:], prm["b_subln_g"][l:l + 1, :].partition_broadcast(128), w=[t_mp])
                    dma("sp", prow[0:31, :], prm["c_conv_w"][l], w=[t_mp])
                    dma("sp", prow[31:32, :], prm["c_conv_b"][l:l + 1, :], w=[t_mp])
                    dma("sp", prow[32:33, :], prm["c_ln_g"][l:l + 1, :], w=[t_mp])
                    dma("sp", prow[33:34, :], prm["c_ln_b"][l:l + 1, :], w=[t_mp])
                    dma("sp", prow[34:37, :], prm["d_conv_w"][l], w=[t_mp])
                    ts("dve", gqk[:, 0:1], gqk[:, 0:1], 32.0 ** -0.5, None, ALU.mult, r=[t_mp], w=[t_mp])
                    tt("dve", lamv[:, 0, :], lamv[:, 0, :], lamv[:, 1, :], ALU.mult, r=[t_mp], w=[t_mp])
                    tt("dve", lamv[:, 2, :], lamv[:, 2, :], lamv[:, 3, :], ALU.mult, r=[t_mp], w=[t_mp])
                    act(lamv[:, 1, :], lamv[:, 0, :], AF.Identity, r=[t_mp], w=[t_mp], accum=lams[:, 0:1])
                    act(lamv[:, 3, :], lamv[:, 2, :], AF.Identity, r=[t_mp], w=[t_mp], accum=lams[:, 1:2])
                    act(lams[:, 2:4], lams[:, 0:2], AF.Exp, r=[t_mp], w=[t_mp])
                    tt("dve", lams[:, 4:5], lams[:, 3:4], lams[:, 2:3], ALU.subtract, r=[t_mp], w=[t_mp])
                    ts("dve", lams[:, 4:5], lams[:, 4:5], -lam_init, None, ALU.add, r=[t_mp], w=[t_mp])
                    ts("dve", sublnb[:], sublnb[:], 1.0 - lam_init, None, ALU.mult, r=[t_mp], w=[t_mp])
                    with ExitStack() as tmp:
                        wsf = sb(tmp, f"wsf{l}", [128, 4, 128], F32)
                        t_wsf = T("wsf")
                        for h in range(4):
                            dma("sp", wsf[:, h, :], prm["a_ws"][l, h], w=[t_wsf])
                        for h in range(4):
                            tr(ps[h][:, 0:128], wsf[:, h, :], ident_f[:], r=[t_wsf, t_const], w=[tps[h]])
                            cp("dve", wsT[:, h, :], ps[h][:, 0:128], r=[tps[h]], w=[t_mp])
                        for c in range(2):
                            tr(ps[4 + c][:, 0:37], prow[:, c * 128:(c + 1) * 128], ident_f[0:37, 0:37], r=[t_mp, t_const], w=[tps[4 + c]])
                            cp("dve", pcol[:, c, :], ps[4 + c][:, 0:37], r=[tps[4 + c]], w=[t_mp])
                        P.barrier()
                    for c in range(2):
                        for k in range(31):
                            ts("pool" if (k % 2) else "dve", diag[:, c, k, :], ident_b[:], pcol[:, c, k:k + 1], None, ALU.mult,
                               r=[t_identb, t_mp], w=[t_diag])
                    if stop == "params":
                        return

                    for b in range(NB):
                        with ExitStack() as bs:
                            qT = sb(bs, f"qT{l}{b}", [64, 4, SEQ], BF16)
                            kT = sb(bs, f"kT{l}{b}", [64, 4, SEQ + CTX], BF16)
                            Vaug = sb(bs, f"Va{l}{b}", [128, 18, 4, 65], BF16)
                            zbuf = sb(bs, f"zb{l}{b}", [128, 2, SEQ + 30], BF16)
                            ebuf = sb(bs, f"eb{l}{b}", [128, 2, SEQ + 2], BF16)
                            yT = sb(bs, f"yT{l}{b}", [128, 8, SEQ], BF16)
                            t_q = [[T() for _ in range(4)] for _ in range(4)]
                            t_k = [[T() for _ in range(5)] for _ in range(4)]
                            t_v = [T() for _ in range(18)]
                            t_z = [[T() for _ in range(5)] for _ in range(2)]
                            t_e = [[T() for _ in range(5)] for _ in range(2)]
                            t_y = [[T() for _ in range(4)] for _ in range(8)]
                            mset("pool", Vaug[:, :, :, 64:65], 1.0, w=t_v)
                            for sq_ in (seqs[2 * b], seqs[2 * b + 1]):
                                n = sq_["n"]
                                isctx = sq_["ctx"]
                                mv = sq_["mv"]
                                src, t_src = seq_src(sq_, l)
                                rsrc = [t_src] if t_src is not None else []
                                kv_only = isctx and last
                                ngr = max(1, n // 512)
                                N = min(n, 512)
                                ntile = N // 128
                                koff = SEQ if isctx else 0
                                ktile0 = 16 if isctx else 0
                                if not kv_only:
                                    for c in range(2):
                                        mset("pool", zbuf[:, c, 0:15], 0.0, w=[t_z[c][4]])
                                        mset("pool", zbuf[:, c, 15 + n:30 + n], 0.0, w=[t_z[c][4]])
                                        mset("pool", ebuf[:, c, 0:1], 0.0, w=[t_e[c][4]])
                                        mset("pool", ebuf[:, c, 1 + n:2 + n], 0.0, w=[t_e[c][4]])
                                for pas in range(1 if kv_only else 2):
                                    with ExitStack() as p1:
                                        win = sb(p1, f"win{l}{b}{int(isctx)}{pas}", [128, 8, 1280], BF16)
                                        t_win = T("win")
                                        dma("pool", win[:], prm["w_in"][l][:, pas * 1280:(pas + 1) * 1280].rearrange("(k p) f -> p k f", p=128),
                                            w=[t_win])
                                        hT = [sb(p1, f"hT{i}", [128, 8, 512], BF16) for i in range(2)]
                                        t_hT = [T(), T()]
                                        xt = [sb(p1, f"xt{i}", [128, D], F32) for i in range(2)]
                                        t_xt = [T(), T()]
                                        junk = sb(p1, "junk", [128, D], BF16)
                                        t_junk = T()
                                        ssn = [sb(p1, f"ssn{i}", [128, 4], F32) for i in range(2)]
                                        t_ssn = [T(), T()]
                                        tmpf = [sb(p1, f"tmpf{i}", [128, 512], F32) for i in range(8)]
                                        t_tmpf = [T() for _ in range(8)]
                                        uT = [sb(p1, f"uT{i}", [128, 2, 512], F32) for i in range(2)]
                                        t_uT = [[T(), T()], [T(), T()]]
                                        vn = [sb(p1, f"vn{i}", [128, 256], BF16) for i in range(2)]
                                        t_vn = [T(), T()]
                                        ssv = [sb(p1, f"ssv{i}", [128, 12], F32) for i in range(2)]
                                        t_ssv = [T(), T()]
                                        ring = Ring([4, 5, 6, 7])
                                        tcount = 0
                                        tmpi = 0
                                        for g in range(ngr):
                                            hTg, t_hTg = hT[g % 2], t_hT[g % 2]
                                            tok0 = g * 512
                                            for t in range(ntile):
                                                par = tcount % 2
                                                tcount += 1
                                                xtt, t_x = xt[par], t_xt[par]
                                                dma("sp", xtt[:], src[tok0 + t * 128: tok0 + (t + 1) * 128, :], r=rsrc, w=[t_x])
                                                rstd = rms_rstd((junk, t_junk, ssn[par], t_ssn[par]), xtt[:], t_x, D, None)
                                                ts("dve", xtt[:], xtt[:], rstd, None, ALU.mult, r=[t_x, t_ssn[par]], w=[t_x])
                                                pA, tpA = ps[2 * par], tps[2 * par]
                                                pB, tpB = ps[2 * par + 1], tps[2 * par + 1]
                                                for k in range(8):
                                                    pp, tpp = (pA, tpA) if k < 4 else (pB, tpB)
                                                    tr(pp[:, (k % 4) * 128:(k % 4 + 1) * 128], xtt[:, k * 128:(k + 1) * 128], ident_f[:],
                                                       r=[t_x, t_const], w=[tpp])
                                                for k in range(8):
                                                    pp, tpp = (pA, tpA) if k < 4 else (pB, tpB)
                                                    o = hTg[:, k, t * 128:(t + 1) * 128]
                                                    i_ = pp[:, (k % 4) * 128:(k % 4 + 1) * 128]
                                                    if k % 2 == 0:
                                                        act(o, i_, AF.Identity, r=[tpp, t_lp], w=[t_hTg], bias=AB[:, 1, mv, k:k + 1],
                                                            scale=AB[:, 0, mv, k:k + 1])
                                                    else:
                                                        ts("dve", o, i_, AB[:, 0, mv, k:k + 1], AB[:, 1, mv, k:k + 1], ALU.mult, ALU.add,
                                                           r=[tpp, t_lp], w=[t_hTg])

                                            def proj(col0, M, tag=None):
                                                pb, tpb = ring.next()
                                                for k in range(8):
                                                    mm(pb[0:M, 0:N], win[:, k, col0:col0 + M], hTg[:, k, 0:N], k == 0, k == 7,
                                                       r=[t_win, t_hTg], w=[tpb])
                                                return pb, tpb

                                            if pas == 0:
                                                if not kv_only:
                                                    for c in range(2):
                                                        pb, tpb = proj(c * 128, 128)
                                                        act(uT[g % 2][:, c, 0:N], pb[:, 0:N], AF.Gelu_apprx_tanh, r=[tpb], w=[t_uT[g % 2][c]])
                                                for which in ((0, 1) if not kv_only else (1,)):
                                                    for h in range(4):
                                                        pb, tpb = proj(512 + which * 256 + h * 64, 64)
                                                        a0, a1, a2, a3 = [(tmpi + j) % 8 for j in range(4)]
                                                        tmpi += 4
                                                        qf, sqb, t1, t2 = tmpf[a0], tmpf[a1], tmpf[a2], tmpf[a3]
                                                        tqf, tsq, tt1, tt2 = t_tmpf[a0], t_tmpf[a1], t_tmpf[a2], t_tmpf[a3]
                                                        act(qf[0:64, 0:N], pb[0:64, 0:N], AF.Identity, r=[tpb], w=[tqf])
                                                        act(sqb[0:64, 0:N], pb[0:64, 0:N], AF.Square, r=[tpb], w=[tsq])
                                                        p2, tp2 = ring.next()
                                                        mm(p2[0:64, 0:N], blk64[:], sqb[0:64, 0:N], True, True, r=[t_const, tsq], w=[tp2])
                                                        act(sqb[0:64, 0:N], p2[0:64, 0:N], AF.Sqrt, r=[tp2], w=[tsq], bias=EPS, scale=1.0 / 32.0)
                                                        rcp(sqb[0:64, 0:N], sqb[0:64, 0:N], r=[tsq], w=[tsq])
                                                        stt(qf[0:64, 0:N], qf[0:64, 0:N], gqk[:, which:which + 1], sqb[0:64, 0:N], ALU.mult, ALU.mult,
                                                            r=[tqf, tsq, t_mp], w=[tqf])
                                                        if which == 0:
                                                            dst = qT[:, h, tok0:tok0 + N]
                                                            tdst = t_q[h][g]
                                                        else:
                                                            dst = kT[:, h, koff + tok0: koff + tok0 + N]
                                                            tdst = t_k[h][4 if isctx else g]
                                                        if not isctx:
                                                            p3, tp3 = ring.next()
                                                            mm(p3[0:64, 0:N], perm64[:], qf[0:64, 0:N], True, True, r=[t_const, tqf], w=[tp3])
                                                            tt("dve", t2[0:64, 0:N], p3[0:64, 0:N], ropes[:, tok0:tok0 + N], ALU.mult, r=[tp3, t_mp], w=[tt2])
                                                            tt("pool", t1[0:64, 0:N], qf[0:64, 0:N], ropec[:, tok0:tok0 + N], ALU.mult, r=[tqf, t_mp], w=[tt1])
                                                            tt("pool", dst, t1[0:64, 0:N], t2[0:64, 0:N], ALU.add, r=[tt1, tt2], w=[tdst])
                                                        else:
                                                            cp("pool", dst, qf[0:64, 0:N], r=[tqf], w=[tdst])
                                                for t in range(ntile):
                                                    pb, tpb = ring.next()
                                                    if not kv_only:
                                                        for k in range(8):
                                                            mm(pb[:, 0:256], hTg[:, k, t * 128:(t + 1) * 128], win[:, k, 256:512], k == 0, k == 7,
                                                               r=[t_win, t_hTg], w=[tpb])
                                                    for k in range(8):
                                                        mm(pb[:, 256:512], hTg[:, k, t * 128:(t + 1) * 128], win[:, k, 1024:1280], k == 0, k == 7,
                                                           r=[t_win, t_hTg], w=[tpb])
                                                    kti = ktile0 + g * 4 + t
                                                    cp("act", Vaug[:, kti, :, 0:64], pb[:, 256:512].rearrange("p (h e) -> p h e", h=4),
                                                       r=[tpb], w=[t_v[kti]])
                                                    if kv_only:
                                                        continue
                                                    vp = (g * 4 + t) % 2
                                                    a0 = tmpi % 8
                                                    tmpi += 1
                                                    vg, tvg = tmpf[a0], t_tmpf[a0]
                                                    act(vg[:, 0:256], pb[:, 0:256], AF.Gelu_apprx_tanh, r=[tpb], w=[tvg])
                                                    for h in range(4):
                                                        act(junk[:, h * 64:(h + 1) * 64], vg[:, h * 64:(h + 1) * 64], AF.Square, r=[tvg],
                                                            w=[t_junk, t_ssv[vp]], accum=ssv[vp][:, h:h + 1])
                                                    act(ssv[vp][:, 4:8], ssv[vp][:, 0:4], AF.Sqrt, r=[t_ssv[vp]], w=[t_ssv[vp]], bias=EPS, scale=1.0 / 64.0)
                                                    rcp(ssv[vp][:, 8:12], ssv[vp][:, 4:8], r=[t_ssv[vp]], w=[t_ssv[vp]])
                                                    for h in range(4):
                                                        stt(vn[vp][:, h * 64:(h + 1) * 64], vg[:, h * 64:(h + 1) * 64], ssv[vp][:, 8 + h:9 + h],
                                                            vngb[:, h * 64:(h + 1) * 64], ALU.mult, ALU.mult, r=[tvg, t_ssv[vp], t_mp], w=[t_vn[vp]])
                                                    for c in range(2):
                                                        pm_, tpm = ring.next()
                                                        for hh in range(2):
                                                            mm(pm_[:, hh * 128:(hh + 1) * 128], vn[vp][:, c * 128:(c + 1) * 128], wsT[:, 2 * c + hh, :], True, True,
                                                               r=[t_vn[vp], t_mp], w=[tpm])
                                                        a1 = tmpi % 8
                                                        tmpi += 1
                                                        mt, tmt = tmpf[a1], t_tmpf[a1]
                                                        for hh in range(2):
                                                            rows = slice(hh * 64, (hh + 1) * 64)
                                                            tt("dve", mt[rows, 0:128], pm_[rows, hh * 128:(hh + 1) * 128],
                                                               bsb[rows, (2 * c + hh) * 128:(2 * c + hh + 1) * 128], ALU.add, r=[tpm, t_mp], w=[tmt])
                                                        tt("pool", yT[:, c, tok0 + t * 128: tok0 + (t + 1) * 128], mt[:, 0:128],
                                                           uT[g % 2][:, c, t * 128:(t + 1) * 128], ALU.mult, r=[tmt, t_uT[g % 2][c]], w=[t_y[c][g]])
                                            else:
                                                for c in range(2):
                                                    pca, tpca = proj(0 + c * 128, 128)
                                                    pcg, tpcg = proj(256 + c * 128, 128)
                                                    a0 = tmpi % 8
                                                    tmpi += 1
                                                    sg, tsg = tmpf[a0], t_tmpf[a0]
                                                    act(sg[:, 0:N], pcg[:, 0:N], AF.Sigmoid, r=[tpcg], w=[tsg])
                                                    tt("dve", zbuf[:, c, 15 + tok0:15 + tok0 + N], pca[:, 0:N], sg[:, 0:N], ALU.mult, r=[tpca, tsg], w=[t_z[c][g]])
                                                for c in range(2):
                                                    pdb, tpdb = proj(512 + c * 128, 128)
                                                    cp("act", yT[:, 6 + c, tok0:tok0 + N], pdb[:, 0:N], r=[tpdb], w=[t_y[6 + c][g]])
                                                    pdc, tpdc = proj(768 + c * 128, 128)
                                                    pdh, tpdh = proj(1024 + c * 128, 128)
                                                    a0 = tmpi % 8
                                                    tmpi += 1
                                                    th, tth = tmpf[a0], t_tmpf[a0]
                                                    cp("act", th[:, 0:N], pdh[:, 0:N], r=[tpdh], w=[tth])
                                                    tt("dve", ebuf[:, c, 1 + tok0:1 + tok0 + N], pdc[:, 0:N], th[:, 0:N], ALU.mult, r=[tpdc, tth], w=[t_e[c][g]])
                                        P.barrier()
                                    if stop == f"p1{pas}":
                                        return
                                if kv_only:
                                    continue
                                with ExitStack() as p2s:
                                    Eb = [sb(p2s, f"Eb{i}", [128, 512], BF16) for i in range(4)]
                                    t_Eb = [T() for _ in range(4)]
                                    ybuf = [sb(p2s, f"ybuf{i}", [128, 4, 256], F32) for i in range(2)]
                                    t_yb = [T(), T()]
                                    ssb = [sb(p2s, f"ssb{i}", [128, 4, 12], F32) for i in range(2)]
                                    t_ssb = [T(), T()]
                                    rr = [sb(p2s, f"rr{i}", [128, 4], F32) for i in range(4)]
                                    t_rr = [T() for _ in range(4)]
                                    tq = [sb(p2s, f"tq{i}", [128, 64], F32) for i in range(4)]
                                    t_tq = [T() for _ in range(4)]
                                    ybn = [sb(p2s, f"ybn{i}", [128, 256], F32) for i in range(2)]
                                    t_ybn = [T(), T()]
                                    junk2 = sb(p2s, "junk2", [128, 512], BF16)
                                    t_junk2 = T()
                                    cf = [sb(p2s, f"cf{i}", [128, 512], F32) for i in range(8)]
                                    t_cf = [T() for _ in range(8)]
                                    ktiles = [16, 17] if isctx else list(range(18))
                                    sring = Ring([2, 3, 4, 5])
                                    oT = [[sb(p2s, f"oT{i}{m}", [65, 512], F32) for m in range(2)] for i in range(2)]
                                    t_oT = [[T(), T()], [T(), T()]]
                                    ei = 0
                                    it = 0
                                    ri = 0
                                    for g in range(ngr):
                                        tok0 = g * 512
                                        yb_, tyb = ybuf[g % 2], t_yb[g % 2]
                                        sb_, tsb = ssb[g % 2], t_ssb[g % 2]
                                        for h in range(4):
                                            pv = [(ps[m], tps[m]) for m in range(2)]
                                            oT_, toT_ = oT[it % 2], t_oT[it % 2]
                                            it += 1
                                            its = [(ki, kt, m) for ki, kt in enumerate(ktiles) for m in range(2)]

                                            def emit_score(ix):
                                                ki, kt, m = its[ix]
                                                kg = 4 if kt >= 16 else kt // 4
                                                psS, tpS = sring.next()
                                                mm(psS[:, 0:N], kT[m * 32:(m + 1) * 32, h, kt * 128:(kt + 1) * 128],
                                                   qT[m * 32:(m + 1) * 32, h, tok0:tok0 + N], True, True, r=[t_k[h][kg], t_q[h][g]], w=[tpS])
                                                return psS, tpS

                                            issued = []
                                            nxi = 0
                                            for ix, (ki, kt, m) in enumerate(its):
                                                while nxi < len(its) and nxi <= ix + 2:
                                                    issued.append(emit_score(nxi))
                                                    nxi += 1
                                                psS, tpS = issued.pop(0)
                                                E, tE = Eb[ei % 4], t_Eb[ei % 4]
                                                ei += 1
                                                act(E[:, 0:N], psS[:, 0:N], AF.Exp, r=[tpS], w=[tE])
                                                mm(pv[m][0][0:65, 0:N], Vaug[:, kt, h, :], E[:, 0:N], ki == 0, ki == len(ktiles) - 1,
                                                   r=[tE, t_v[kt]], w=[pv[m][1]])
                                            if bp_hit():
                                                P.barrier()
                                                return
                                            for m in range(2):
                                                cp("act" if m == 0 else "dve", oT_[m][:, 0:N], pv[m][0][0:65, 0:N], r=[pv[m][1]], w=[toT_[m]])
                                            if bp_hit():
                                                P.barrier()
                                                return
                                            for qt in range(ntile):
                                                for m in range(2):
                                                    tr(ps[6 + m][:, qt * 65:(qt + 1) * 65], oT_[m][0:65, qt * 128:(qt + 1) * 128], ident_f[0:65, 0:65],
                                                       r=[toT_[m], t_const], w=[tps[6 + m]])
                                            if bp_hit():
                                                P.barrier()
                                                return
                                            for qt in range(ntile):
                                                r_, tr_ = rr[ri % 4], t_rr[ri % 4]
                                                tq_, ttq = tq[ri % 4], t_tq[ri % 4]
                                                ri += 1
                                                rcp(r_[:, 0:1], ps[6][:, qt * 65 + 64: qt * 65 + 65], r=[tps[6]], w=[tr_])
                                                rcp(r_[:, 1:2], ps[7][:, qt * 65 + 64: qt * 65 + 65], r=[tps[7]], w=[tr_])
                                                tt("dve", r_[:, 2:3], r_[:, 1:2], lams[:, 4:5], ALU.mult, r=[tr_, t_mp], w=[tr_])
                                                ts("dve", tq_[:], ps[6][:, qt * 65: qt * 65 + 64], r_[:, 0:1], None, ALU.mult, r=[tps[6], tr_], w=[ttq])
                                                stt(yb_[:, qt, h * 64:(h + 1) * 64], ps[7][:, qt * 65: qt * 65 + 64], r_[:, 2:3], tq_[:], ALU.mult, ALU.add,
                                                    r=[tps[7], tr_, ttq], w=[tyb])
                                                act(junk2[:, 0:64], yb_[:, qt, h * 64:(h + 1) * 64], AF.Square, r=[tyb], w=[t_junk2, tsb],
                                                    accum=sb_[:, qt, h:h + 1])
                                            if bp_hit():
                                                P.barrier()
                                                return
                                        for qt in range(ntile):
                                            act(sb_[:, qt, 4:8], sb_[:, qt, 0:4], AF.Sqrt, r=[tsb], w=[tsb], bias=EPS, scale=1.0 / 64.0)
                                            rcp(sb_[:, qt, 8:12], sb_[:, qt, 4:8], r=[tsb], w=[tsb])
                                            if bp_hit():
                                                P.barrier()
                                                return
                                            yn, tyn = ybn[qt % 2], t_ybn[qt % 2]
                                            for h in range(4):
                                                stt(yn[:, h * 64:(h + 1) * 64], yb_[:, qt, h * 64:(h + 1) * 64], sb_[:, qt, 8 + h:9 + h], sublnb[:],
                                                    ALU.mult, ALU.mult, r=[tyb, tsb, t_mp], w=[tyn])
                                            if bp_hit():
                                                P.barrier()
                                                return
                                            for cc in range(2):
                                                tr(ps[6][:, cc * 128:(cc + 1) * 128], yn[:, cc * 128:(cc + 1) * 128], ident_f[:], r=[tyn, t_const], w=[tps[6]])
                                            if bp_hit():
                                                P.barrier()
                                                return
                                            for cc in range(2):
                                                cp("act", yT[:, 2 + cc, tok0 + qt * 128: tok0 + (qt + 1) * 128], ps[6][:, cc * 128:(cc + 1) * 128],
                                                   r=[tps[6]], w=[t_y[2 + cc][g]])
                                                if bp_hit():
                                                    P.barrier()
                                                    return
                                    if stop == "p2a":
                                        P.barrier()
                                        return
                                    cring = Ring([4, 5, 6, 7])
                                    ci = 0
                                    for g in range(ngr):
                                        tok0 = g * 512
                                        zc, tzc, zq, tzq = [], [], [], []
                                        for c in range(2):
                                            pcv, tpcv = cring.next()
                                            for k in range(31):
                                                mm(pcv[:, 0:N], diag[:, c, k, :], zbuf[:, c, tok0 + k: tok0 + k + N], k == 0, k == 30,
                                                   r=[t_diag] + t_z[c], w=[tpcv])
                                            a, b2 = cf[ci % 8], cf[(ci + 1) % 8]
                                            ta, tb2 = t_cf[ci % 8], t_cf[(ci + 1) % 8]
                                            ci += 2
                                            act(a[:, 0:N], pcv[:, 0:N], AF.Identity, r=[tpcv, t_mp], w=[ta], bias=pcol[:, c, 31:32])
                                            act(b2[:, 0:N], a[:, 0:N], AF.Square, r=[ta], w=[tb2])
                                            zc.append(a); tzc.append(ta); zq.append(b2); tzq.append(tb2)
                                        pme, tpme = cring.next()
                                        pms, tpms = cring.next()
                                        for c in range(2):
                                            mm(pme[:, 0:N], ones256[:], zc[c][:, 0:N], c == 0, c == 1, r=[t_ones, tzc[c]], w=[tpme])
                                        for c in range(2):
                                            mm(pms[:, 0:N], ones256[:], zq[c][:, 0:N], c == 0, c == 1, r=[t_ones, tzq[c]], w=[tpms])
                                        m2, tm2 = cf[ci % 8], t_cf[ci % 8]
                                        ci += 1
                                        act(m2[:, 0:N], pme[:, 0:N], AF.Square, r=[tpme], w=[tm2])
                                        tt("dve", m2[:, 0:N], pms[:, 0:N], m2[:, 0:N], ALU.subtract, r=[tpms, tm2], w=[tm2])
                                        ts("dve", m2[:, 0:N], m2[:, 0:N], 0.0, None, ALU.max, r=[tm2], w=[tm2])
                                        act(m2[:, 0:N], m2[:, 0:N], AF.Sqrt, r=[tm2], w=[tm2], bias=EPS, scale=1.0)
                                        rcp(m2[:, 0:N], m2[:, 0:N], r=[tm2], w=[tm2])
                                        for c in range(2):
                                            tt("dve", zc[c][:, 0:N], zc[c][:, 0:N], pme[:, 0:N], ALU.subtract, r=[tzc[c], tpme], w=[tzc[c]])
                                            tt("pool", zc[c][:, 0:N], zc[c][:, 0:N], m2[:, 0:N], ALU.mult, r=[tzc[c], tm2], w=[tzc[c]])
                                            act(yT[:, 4 + c, tok0:tok0 + N], zc[c][:, 0:N], AF.Silu, r=[tzc[c], t_mp], w=[t_y[4 + c][g]],
                                                bias=pcol[:, c, 33:34], scale=pcol[:, c, 32:33])
                                    if stop == "p2b":
                                        P.barrier()
                                        return
                                    for g in range(ngr):
                                        tok0 = g * 512
                                        for c in range(2):
                                            a, ta = cf[ci % 8], t_cf[ci % 8]
                                            ci += 1
                                            ts("dve", a[:, 0:N], ebuf[:, c, tok0:tok0 + N], pcol[:, c, 34:35], None, ALU.mult, r=t_e[c] + [t_mp], w=[ta])
                                            stt(a[:, 0:N], ebuf[:, c, tok0 + 1:tok0 + 1 + N], pcol[:, c, 35:36], a[:, 0:N], ALU.mult, ALU.add, r=t_e[c] + [ta], w=[ta])
                                            stt(a[:, 0:N], ebuf[:, c, tok0 + 2:tok0 + 2 + N], pcol[:, c, 36:37], a[:, 0:N], ALU.mult, ALU.add, r=t_e[c] + [ta], w=[ta])
                                            tt("pool", yT[:, 6 + c, tok0:tok0 + N], yT[:, 6 + c, tok0:tok0 + N], a[:, 0:N], ALU.mult, r=[t_y[6 + c][g], ta],
                                               w=[t_y[6 + c][g]])
                                    P.barrier()
                                    if stop == "p2":
                                        return
                                with ExitStack() as p3s:
                                    wout = sb(p3s, "wout", [128, 8, D], BF16)
                                    t_wout = T()
                                    dma("pool", wout[:], prm["w_out"][l].rearrange("(k p) f -> p k f", p=128), w=[t_wout])
                                    g1b = sb(p3s, "g1b", [128, D], F32)
                                    t_g1b = T()
                                    dma("sp", g1b[:], mod_d[l, mv:mv + 1, 2 * D:3 * D].partition_broadcast(128), r=[t_mod], w=[t_g1b])
                                    xt3 = [sb(p3s, f"xt3{i}", [128, D], F32) for i in range(2)]
                                    t_xt3 = [T(), T()]
                                    xo = [sb(p3s, f"xo{i}", [128, D], F32) for i in range(2)]
                                    t_xo = [T(), T()]
                                    for ti in range(n // 128):
                                        g = ti // 4
                                        par = ti % 2
                                        dma("sp", xt3[par][:], src[ti * 128:(ti + 1) * 128, :], r=rsrc, w=[t_xt3[par]])
                                        for dh in range(2):
                                            pb, tpb = ps[par * 2 + dh], tps[par * 2 + dh]
                                            for j in range(8):
                                                mm(pb[:, :], yT[:, j, ti * 128:(ti + 1) * 128], wout[:, j, dh * 512:(dh + 1) * 512], j == 0, j == 7,
                                                   r=[t_y[j][g], t_wout], w=[tpb])
                                            tt("dve", xo[par][:, dh * 512:(dh + 1) * 512], pb[:, :], g1b[:, dh * 512:(dh + 1) * 512], ALU.mult,
                                               r=[tpb, t_g1b], w=[t_xo[par]])
                                        tt("pool", xo[par][:], xo[par][:], xt3[par][:], ALU.add, r=[t_xo[par], t_xt3[par]], w=[t_xo[par]])
                                        dma("sp", xa_d[sq_["off"] + ti * 128: sq_["off"] + (ti + 1) * 128, :], xo[par][:], r=[t_xo[par]], w=[t_xa])
                                    P.barrier()
                                    if stop == "p3":
                                        dma("sp", dbg_d["dbg_xa"], xa_d, r=[t_xa], w=[T("dbg")])
                                        return
                    P.barrier()

                if dbg and "dbg_xa" in dbg_d and l == 0:
                    dma("sp", dbg_d["dbg_xa"], xa_d, r=[t_xa], w=[T("dbg")])
                    dma("sp", dbg_d["dbg_mod"], mod_d[0], r=[t_mod], w=[T("dbg2")])
                if skip_moe:
                    P.barrier()
                    continue
                if MOE_SPARSE:
                    tl_all = []
                    for s in seqs:
                        if s["ctx"] and not last:
                            for ti in range(s["n"] // 128):
                                r0 = s["off"] + ti * 128
                                tl_all.append((r0, xb_d[r0:r0 + 128, :], t_xb, 2))
                    for s in seqs:
                        if s["ctx"]:
                            continue
                        for ti in range(s["n"] // 128):
                            r0 = s["off"] + ti * 128
                            if last:
                                tl_all.append((r0, out_d[s["b"], ti * 128:(ti + 1) * 128, :], t_out, s["mv"]))
                            else:
                                tl_all.append((r0, xb_d[r0:r0 + 128, :], t_xb, s["mv"]))
                    NTT = len(tl_all)
                    NT = (NTT * 128 * 4) // 512 + NEXP
                    t_h2d, t_xp, t_yd = T("h2d"), T("xp"), T("yd")
                    with ExitStack() as me:
                        rw = sb(me, "rw", [128, 8, NEXP], F32)
                        rbb = sb(me, "rbb", [128, NEXP], F32)
                        bdn = sb(me, "bdn", [NEXP, D], F32)
                        bgu_b = sb(me, "bgub", [NEXP, 2 * D], BF16)
                        ltri = sb(me, "ltri", [128, 128], F32)
                        onesf = sb(me, "onesf", [128, 128], F32)
                        eiota = sb(me, "eiota", [128, 1], F32)
                        pk = sb(me, "pk", [128, 8], F32)
                        gates_all = sb(me, "gates_all", [128, NTT, NEXP], F32)
                        sidx = sb(me, "sidx", [128, NTT, 4], U32)
                        gk = sb(me, "gk", [128, NTT, 4], F32)
                        idxu = sb(me, "idxu", [128, NT, 8], U32)
                        oh = sb(me, "oh", [NEXP, NT], F32)
                        t_me, t_ga, t_sidx, t_gk, t_idx, t_oh = T(), T(), T(), T(), T(), T()
                        dma("sp", rw[:], prm["router_w"][l].rearrange("(k p) e -> p k e", p=128), w=[t_me])
                        dma("sp", rbb[:], prm["router_b"][l:l + 1, :].partition_broadcast(128), w=[t_me])
                        dma("sp", bdn[:], prm["exp_b_dn"][l], w=[t_me])
                        dma("pool", bgu_b[:], prm["exp_b_gu"][l], w=[t_me])
                        dma("sp", ltri[:], ltri_in, w=[t_me])
                        dma("sp", eiota[:], eiota_in, w=[t_me])
                        dma("sp", pk[:], pk_in[:, l * 8:(l + 1) * 8], w=[t_me])
                        mset("pool", onesf[:], 1.0, w=[t_me])
                        with ExitStack() as sa:
                            mask_all = sb(sa, "mask_all", [128, NTT, NEXP], F32)
                            rank_all = sb(sa, "rank_all", [128, NTT, NEXP], F32)
                            Ssum = sb(sa, "Ssum", [128, NEXP], F32)
                            t_mk, t_rk, t_S = T(), T(), T()
                            mset("dve", Ssum[:], 0.0, w=[t_S])
                            xm = [sb(sa, f"xm{i}", [128, D], F32) for i in range(2)]
                            t_xm = [T(), T()]
                            h2t = [sb(sa, f"h2t{i}", [128, D], F32) for i in range(2)]
                            t_h2t = [T(), T()]
                            h2f = [sb(sa, f"h2f{i}", [128, 8, 128], F32) for i in range(2)]
                            t_h2f = [T(), T()]
                            junkm = sb(sa, "junkm", [128, D], BF16)
                            t_junkm = T()
                            ssm = [sb(sa, f"ssm{i}", [128, 4], F32) for i in range(2)]
                            t_ssm = [T(), T()]
                            rt = [sb(sa, f"rt{i}", [128, 4, NEXP], F32) for i in range(2)]
                            t_rt = [T(), T()]
                            rs8 = [sb(sa, f"rs8{i}", [128, 12], F32) for i in range(2)]
                            A2b = sb(sa, "A2b", [128, D], F32)
                            B2b = sb(sa, "B2b", [128, D], F32)
                            n2gb = sb(sa, "n2gb", [128, D], F32)
                            t_ab = T()
                            dma("sp", n2gb[:], prm["norm2_g"][l:l + 1, :].partition_broadcast(128), w=[t_ab])
                            cur_mv_a = [None]

                            def front_a(ti):
                                r0, dst, tdst, mv = tl_all[ti]
                                par = ti % 2
                                if mv != cur_mv_a[0]:
                                    cur_mv_a[0] = mv
                                    dma("sp", A2b[:], mod_d[l, mv:mv + 1, 4 * D:5 * D].partition_broadcast(128), r=[t_mod], w=[t_ab])
                                    dma("sp", B2b[:], mod_d[l, mv:mv + 1, 3 * D:4 * D].partition_broadcast(128), r=[t_mod], w=[t_ab])
                                    stt(A2b[:], A2b[:], 1.0, n2gb[:], ALU.add, ALU.mult, r=[t_ab], w=[t_ab])
                                xtt, t_x = xm[par], t_xm[par]
                                dma("sp", xtt[:], xa_d[r0:r0 + 128, :], r=[t_xa], w=[t_x])
                                rstd = rms_rstd((junkm, t_junkm, ssm[par], t_ssm[par]), xtt[:], t_x, D, None)
                                ht, tht = h2t[par], t_h2t[par]
                                stt(ht[:], xtt[:], rstd, A2b[:], ALU.mult, ALU.mult, r=[t_x, t_ssm[par], t_ab], w=[tht])
                                tt("pool", ht[:], ht[:], B2b[:], ALU.add, r=[tht, t_ab], w=[tht])
                                dma("sp", h2_d[ti * 128:(ti + 1) * 128, :], ht[:], r=[tht], w=[t_h2d])
                                pA, tpA = ps[2 * par], tps[2 * par]
                                pB, tpB = ps[2 * par + 1], tps[2 * par + 1]
                                for k in range(8):
                                    pp, tpp = (pA, tpA) if k < 4 else (pB, tpB)
                                    tr(pp[:, (k % 4) * 128:(k % 4 + 1) * 128], ht[:, k * 128:(k + 1) * 128], ident_f[:], r=[tht, t_const], w=[tpp])
                                hf_, thf = h2f[par], t_h2f[par]
                                cp("act", hf_[:, 0:4, :], pA[:, :].rearrange("p (k t) -> p k t", k=4), r=[tpA], w=[thf])
                                cp("dve", hf_[:, 4:8, :], pB[:, :].rearrange("p (k t) -> p k t", k=4), r=[tpB], w=[thf])
                                pr, tpr = ps[4 + par], tps[4 + par]
                                for k in range(8):
                                    mm(pr[:, 0:NEXP], hf_[:, k, :], rw[:, k, :], k == 0, k == 7, r=[thf, t_me], w=[tpr])

                            def tail_a(ti):
                                par = ti % 2
                                pr, tpr = ps[4 + par], tps[4 + par]
                                R_, tR = rt[par], t_rt[par]
                                s8 = rs8[par]
                                tt("dve", R_[:, 0, :], pr[:, 0:NEXP], rbb[:], ALU.add, r=[tpr, t_me], w=[tR])
                                P.op("dve", (lambda o, i_: (lambda e: e.max(out=o, in_=i_)))(s8[:, 0:8], R_[:, 0, :]), [tR], [tR])
                                ts("dve", mask_all[:, ti, :], R_[:, 0, :], s8[:, 3:4], None, ALU.is_ge, r=[tR], w=[t_mk])
                                ts("dve", s8[:, 8:9], s8[:, 0:1], -1.0, None, ALU.mult, r=[tR], w=[tR])
                                act(R_[:, 2, :], R_[:, 0, :], AF.Exp, r=[tR], w=[tR], bias=s8[:, 8:9], scale=1.0)
                                tt("dve", R_[:, 2, :], R_[:, 2, :], mask_all[:, ti, :], ALU.mult, r=[tR, t_mk], w=[tR])
                                act(R_[:, 3, :], R_[:, 2, :], AF.Identity, r=[tR], w=[tR], accum=s8[:, 9:10])
                                rcp(s8[:, 10:11], s8[:, 9:10], r=[tR], w=[tR])
                                ts("dve", gates_all[:, ti, :], R_[:, 2, :], s8[:, 10:11], None, ALU.mult, r=[tR], w=[t_ga])
                                pk_, tpk = ps[6 + par], tps[6 + par]
                                mm(pk_[:, 0:NEXP], ltri[:], mask_all[:, ti, :], True, False, r=[t_me, t_mk], w=[tpk])
                                mm(pk_[:, 0:NEXP], onesf[:], Ssum[:], False, True, r=[t_me, t_S], w=[tpk])
                                cp("act", rank_all[:, ti, :], pk_[:, 0:NEXP], r=[tpk], w=[t_rk])
                                tt("dve", Ssum[:], Ssum[:], mask_all[:, ti, :], ALU.add, r=[t_S, t_mk], w=[t_S])

                            front_a(0)
                            for ti in range(NTT):
                                if ti + 1 < NTT:
                                    front_a(ti + 1)
                                tail_a(ti)

                            sm = sb(sa, "sm", [128, 8, NEXP], F32)
                            t_sm = T()
                            mm(ps[0][:, 0:NEXP], onesf[:], Ssum[:], True, True, r=[t_me, t_S], w=[tps[0]])
                            cp("dve", sm[:, 0, :], ps[0][:, 0:NEXP], r=[tps[0]], w=[t_sm])
                            ts("dve", sm[:, 1, :], sm[:, 0, :], 0.0, None, ALU.is_gt, r=[t_sm], w=[t_sm])
                            for i in range(1, 10):
                                stt(sm[:, 1, :], sm[:, 0, :], 512.0 * i, sm[:, 1, :], ALU.is_gt, ALU.add, r=[t_sm], w=[t_sm])
                            cp("dve", sm[:, 2, :], sm[:, 1, :], r=[t_sm], w=[t_sm])
                            src_i = 2
                            for sh in (1, 2, 4, 8, 16):
                                dst_i = 5 - src_i
                                cp("dve", sm[:, dst_i, 0:sh], sm[:, src_i, 0:sh], r=[t_sm], w=[t_sm])
                                tt("dve", sm[:, dst_i, sh:NEXP], sm[:, src_i, sh:NEXP], sm[:, src_i, 0:NEXP - sh], ALU.add, r=[t_sm], w=[t_sm])
                                src_i = dst_i
                            incl = sm[:, src_i, :]
                            tt("dve", sm[:, 4, :], incl, sm[:, 1, :], ALU.subtract, r=[t_sm], w=[t_sm])
                            ts("dve", sm[:, 4, :], sm[:, 4, :], 512.0, 1.0, ALU.mult, ALU.add, r=[t_sm], w=[t_sm])
                            ejb = sb(sa, "ejb", [128, NT], F32)
                            idxf = sb(sa, "idxf", [128, NT, 8], F32)
                            t_ej = T()
                            for j in range(NT):
                                P.op("dve", (lambda o, i_, thr, acc_: (lambda e: e.tensor_scalar(o, i_, thr, 0.0, ALU.is_lt, ALU.add, accum_out=acc_)))(
                                    sm[:, 5, :], incl, j + 0.5, ejb[:, j:j + 1]), [t_sm], [t_sm, t_ej])
                            ts("dve", ejb[:], ejb[:], float(NEXP - 1), None, ALU.min, r=[t_ej], w=[t_ej])
                            ts("dve", oh[:], ejb[0:NEXP, :], eiota[0:NEXP, 0:1], None, ALU.is_equal, r=[t_ej, t_me], w=[t_oh])
                            for k in range(8):
                                ts("dve", idxf[:, :, k], ejb[:], 1024.0, pk[:, k:k + 1], ALU.mult, ALU.add, r=[t_ej, t_me], w=[t_ej])
                            cp("dve", idxu[:], idxf[:], r=[t_ej], w=[t_idx])
                            hb_ = [sb(sa, f"hsc{i}", [128, D], F32) for i in range(4)]
                            t_hb = [T() for _ in range(4)]
                            NSET = 4
                            vals = [sb(sa, f"val{i}", [128, NEXP], F32) for i in range(NSET)]
                            t8s = [sb(sa, f"t8{i}", [128, 8], F32) for i in range(NSET)]
                            eqbs = [sb(sa, f"eqb{i}", [128, NEXP], F32) for i in range(NSET)]
                            sifs = [sb(sa, f"sif{i}", [128, 4], F32) for i in range(NSET)]
                            t_vals = [T() for _ in range(NSET)]

                            def slot_seq(ti, si):
                                val, t8, eqb, sif, t_val = vals[si], t8s[si], eqbs[si], sifs[si], t_vals[si]
                                tt("dve", val[:], rank_all[:, ti, :], sm[:, 4, :], ALU.add, r=[t_rk, t_sm], w=[t_val])
                                yield
                                tt("dve", val[:], val[:], mask_all[:, ti, :], ALU.mult, r=[t_val, t_mk], w=[t_val])
                                yield
                                P.op("dve", (lambda o, i_: (lambda e: e.max(out=o, in_=i_)))(t8[:], val[:]), [t_val], [t_val])
                                yield
                                ts("dve", sif[:], t8[:, 0:4], -1.0, None, ALU.add, r=[t_val], w=[t_val])
                                yield
                                cp("dve", sidx[:, ti, :], sif[:], r=[t_val], w=[t_sidx])
                                yield
                                b_, tb_ = hb_[ti % 4], t_hb[ti % 4]
                                dma("sp", b_[:], h2_d[ti * 128:(ti + 1) * 128, :], r=[t_h2d], w=[tb_])
                                for k in range(4):
                                    P.dma("pool", (lambda o, i_: (lambda e: e.indirect_dma_start(out=xp_d, out_offset=IndirectOffsetOnAxis(ap=o, axis=0), in_=i_, in_offset=None)))(
                                        sidx[:, ti, k:k + 1], b_[:]), [tb_, t_sidx], [t_xp])
                                for k in range(4):
                                    stt(eqb[:], val[:], t8[:, k:k + 1], gates_all[:, ti, :], ALU.is_equal, ALU.mult, r=[t_val, t_ga], w=[t_val])
                                    yield
                                    P.op("dve", (lambda o, i_: (lambda e: e.reduce_sum(o, i_, axis=mybir.AxisListType.X)))(gk[:, ti, k:k + 1], eqb[:]),
                                         [t_val], [t_gk])
                                    yield

                            for base in range(0, NTT, NSET):
                                gens = [slot_seq(ti, ti - base) for ti in range(base, min(NTT, base + NSET))]
                                while gens:
                                    alive = []
                                    for gq in gens:
                                        try:
                                            next(gq)
                                            alive.append(gq)
                                        except StopIteration:
                                            pass
                                    gens = alive
                            P.barrier()
                        with ExitStack() as sd:
                            wgu = [sb(sd, f"wgu{i}", [128, 8, 2 * D], BF16) for i in range(2)]
                            t_wgu = [T(), T()]
                            wdn = sb(sd, "wdn", [128, 8, D], BF16)
                            t_wdn = T()
                            xt_ = [sb(sd, f"xts{i}", [128, D], F32) for i in range(2)]
                            t_xt_ = [T(), T()]
                            h2T = [sb(sd, f"h2T{i}", [128, 8, 512], BF16) for i in range(2)]
                            t_h2T = [T(), T()]
                            hid = [sb(sd, f"hid{i}", [128, 8, 512], BF16) for i in range(2)]
                            t_hid = [[T() for _ in range(8)] for _ in range(2)]
                            sw = [[sb(sd, f"sw{i}{j}", [128, 512], F32) for j in range(3)] for i in range(2)]
                            t_sw = [[T() for _ in range(3)] for _ in range(2)]
                            yo = [sb(sd, f"yo{i}", [128, D], F32) for i in range(2)]
                            t_yo = [T(), T()]
                            ones32 = sb(sd, "ones32", [NEXP, 512], BF16)
                            ohr = [sb(sd, f"ohr{i}", [NEXP, 512], BF16) for i in range(2)]
                            t_ohr = [T(), T()]
                            t_o32 = T()
                            mset("pool", ones32[:], 1.0, w=[t_o32])
                            biasj = [sb(sd, f"biasj{i}", [128, 16], F32) for i in range(2)]
                            t_biasj = [T(), T()]
                            ohb = sb(sd, "ohb", [NEXP, NT], BF16)
                            t_ohb = T()
                            cp("dve", ohb[:], oh[:], r=[t_oh], w=[t_ohb])
                            gring = Ring([2, 3, 4, 5])
                            dring = Ring([6, 7])
                            sub = 0
                            stg = [sb(sd, f"stg{i}", [128, 2 * D], F32) for i in range(4)]
                            t_stg = [T() for _ in range(4)]
                            t_wguk = [[T() for _ in range(8)] for _ in range(2)]
                            t_wdnk = [T() for _ in range(8)]
                            sctr = [0]
                            pending = []

                            def issue_gather(flat, ix, dst_ap, t_dst, ncols, eng):
                                i = sctr[0] % 4
                                sctr[0] += 1
                                P.dma("pool", (lambda o, ixx: (lambda e: e.indirect_dma_start(out=o, out_offset=None, in_=flat, in_offset=IndirectOffsetOnAxis(ap=ixx, axis=0))))(
                                    stg[i][:, 0:ncols], ix), [t_idx], [t_stg[i]])
                                pending.append((i, dst_ap, t_dst, ncols, eng))

                            def flush_casts():
                                while pending:
                                    i, dst_ap, t_dst, ncols, eng = pending.pop(0)
                                    cp(eng, dst_ap, stg[i][:, 0:ncols], r=[t_stg[i]], w=[t_dst])

                            for k in range(8):
                                issue_gather(wgu_flat, idxu[:, 0, k:k + 1], wgu[0][:, k, :], t_wguk[0][k], 2 * D, "act")
                                flush_casts()
                            subc = [0]

                            def emit_tr(jx):
                                hTx, thTx = h2T[jx % 2], t_h2T[jx % 2]
                                for q_ in range(4):
                                    par = subc[0] % 2
                                    subc[0] += 1
                                    dma("sp", xt_[par][:], xp_d[jx * 512 + q_ * 128: jx * 512 + (q_ + 1) * 128, :], r=[t_xp], w=[t_xt_[par]])
                                    for k in range(8):
                                        pp, tpp = (ps[0], tps[0]) if k < 4 else (ps[1], tps[1])
                                        tr(pp[:, (k % 4) * 128:(k % 4 + 1) * 128], xt_[par][:, k * 128:(k + 1) * 128], ident_f[:], r=[t_xt_[par], t_const], w=[tpp])
                                    cp("act", hTx[:, 0:4, q_ * 128:(q_ + 1) * 128], ps[0][:, :].rearrange("p (k t) -> p k t", k=4), r=[tps[0]], w=[thTx])
                                    cp("dve", hTx[:, 4:8, q_ * 128:(q_ + 1) * 128], ps[1][:, :].rearrange("p (k t) -> p k t", k=4), r=[tps[1]], w=[thTx])
                            for j in range(NT):
                                wb, twbk = wgu[j % 2], t_wguk[j % 2]
                                hT_, thT = h2T[j % 2], t_h2T[j % 2]
                                pbz, tpbz = dring.next()
                                for c in range(16):
                                    mm(pbz[:, c:c + 1], bgu_b[:, c * 128:(c + 1) * 128], ohb[:, j:j + 1], True, True, r=[t_me, t_ohb], w=[tpbz])
                                bj, tbj = biasj[j % 2], t_biasj[j % 2]
                                cp("act", bj[:], pbz[:, 0:16], r=[tpbz], w=[tbj])
                                if j == 0:
                                    emit_tr(0)
                                hb, thb = hid[j % 2], t_hid[j % 2]
                                for jj in range(8):
                                    flush_casts()
                                    issue_gather(wdn_flat, idxu[:, j, jj:jj + 1], wdn[:, jj, :], t_wdnk[jj], D, "dve")
                                    if j + 1 < NT:
                                        issue_gather(wgu_flat, idxu[:, j + 1, jj:jj + 1], wgu[(j + 1) % 2][:, jj, :], t_wguk[(j + 1) % 2][jj], 2 * D, "act")
                                    pgl, tpgl = gring.next()
                                    pup, tpup = gring.next()
                                    for k in range(8):
                                        mm(pgl[:, :], wb[:, k, jj * 128:(jj + 1) * 128], hT_[:, k, :], k == 0, k == 7, r=[twbk[k], thT], w=[tpgl])
                                    for k in range(8):
                                        mm(pup[:, :], wb[:, k, D + jj * 128: D + (jj + 1) * 128], hT_[:, k, :], k == 0, k == 7, r=[twbk[k], thT], w=[tpup])
                                    sp_ = jj % 2
                                    a_, s_, u_ = sw[sp_]
                                    ta_, ts_, tu_ = t_sw[sp_]
                                    ts("dve", a_[:], pgl[:, :], bj[:, jj:jj + 1], 7.0, ALU.add, ALU.min, r=[tpgl, tbj], w=[ta_])
                                    act(s_[:], a_[:], AF.Sigmoid, r=[ta_], w=[ts_], scale=1.702)
                                    act(u_[:], pup[:, :], AF.Identity, r=[tpup, tbj], w=[tu_], bias=bj[:, 8 + jj:9 + jj])
                                    ts("dve", u_[:], u_[:], 7.0, -7.0, ALU.min, ALU.max, r=[tu_], w=[tu_])
                                    tt("dve", a_[:], a_[:], s_[:], ALU.mult, r=[ta_, ts_], w=[ta_])
                                    stt(hb[:, jj, :], u_[:], 1.0, a_[:], ALU.add, ALU.mult, r=[ta_, tu_], w=[thb[jj]])
                                flush_casts()
                                if j + 1 < NT:
                                    emit_tr(j + 1)
                                for q_ in range(4):
                                    yy, tyy = yo[q_ % 2], t_yo[q_ % 2]
                                    for dh in range(2):
                                        pd, tpd = dring.next()
                                        for jj in range(8):
                                            mm(pd[:, :], hb[:, jj, q_ * 128:(q_ + 1) * 128], wdn[:, jj, dh * 512:(dh + 1) * 512], jj == 0, jj == 7,
                                               r=[thb[jj], t_wdnk[jj]], w=[tpd])
                                        cp("act" if dh == 0 else "dve", yy[:, dh * 512:(dh + 1) * 512], pd[:, :], r=[tpd], w=[tyy])
                                    dma("sp", y_d[j * 512 + q_ * 128: j * 512 + (q_ + 1) * 128, :], yy[:], r=[tyy], w=[t_yd])
                            P.barrier()
                        with ExitStack() as se:
                            xm = [sb(se, f"xme{i}", [128, D], F32) for i in range(2)]
                            t_xm = [T(), T()]
                            acc = [sb(se, f"acce{i}", [128, D], F32) for i in range(2)]
                            t_acc = [T(), T()]
                            yk = [sb(se, f"yk{i}", [128, D], F32) for i in range(4)]
                            t_yk = [T() for _ in range(4)]
                            gT = [sb(se, f"gTe{i}", [NEXP, 128], F32) for i in range(2)]
                            t_gT = [T(), T()]
                            g2b = sb(se, "g2be", [128, D], F32)
                            t_g2b = T()
                            cur_mv = None
                            yi = 0
                            for ti, (r0, dst, tdst, mv) in enumerate(tl_all):
                                par = ti % 2
                                if mv != cur_mv:
                                    cur_mv = mv
                                    dma("sp", g2b[:], mod_d[l, mv:mv + 1, 5 * D:6 * D].partition_broadcast(128), r=[t_mod], w=[t_g2b])
                                dma("sp", xm[par][:], xa_d[r0:r0 + 128, :], r=[t_xa], w=[t_xm[par]])
                                pg, tpg = ps[4 + par], tps[4 + par]
                                tr(pg[0:NEXP, 0:128], gates_all[:, ti, :], ident_f[:], r=[t_ga, t_const], w=[tpg])
                                cp("act", gT[par][:], pg[0:NEXP, 0:128], r=[tpg], w=[t_gT[par]])
                                for dh in range(2):
                                    pb, tpb = ps[par * 2 + dh], tps[par * 2 + dh]
                                    mm(pb[:, :], gT[par][:], bdn[:, dh * 512:(dh + 1) * 512], True, True, r=[t_gT[par], t_me], w=[tpb])
                                    cp("act", acc[par][:, dh * 512:(dh + 1) * 512], pb[:, :], r=[tpb], w=[t_acc[par]])
                                for k in range(4):
                                    y_, ty_ = yk[yi % 4], t_yk[yi % 4]
                                    yi += 1
                                    P.dma("pool", (lambda o, ix: (lambda e: e.indirect_dma_start(out=o, out_offset=None, in_=y_d, in_offset=IndirectOffsetOnAxis(ap=ix, axis=0))))(
                                        y_[:], sidx[:, ti, k:k + 1]), [t_sidx, t_yd], [ty_])
                                    stt(acc[par][:], y_[:], gk[:, ti, k:k + 1], acc[par][:], ALU.mult, ALU.add, r=[ty_, t_gk, t_acc[par]], w=[t_acc[par]])
                                tt("dve", acc[par][:], acc[par][:], g2b[:], ALU.mult, r=[t_acc[par], t_g2b], w=[t_acc[par]])
                                tt("pool", xm[par][:], xm[par][:], acc[par][:], ALU.add, r=[t_xm[par], t_acc[par]], w=[t_xm[par]])
                                dma("sp", dst, xm[par][:], r=[t_xm[par]], w=[tdst])
                            P.barrier()
                    if dbg and "dbg_xb" in dbg_d and l == 0:
                        dma("sp", dbg_d["dbg_xb"], xb_d, r=[t_xb], w=[T("dbg3")])
                    continue
                with ExitStack() as me:
                    rw = sb(me, f"rw{l}", [128, 8, NEXP], F32)
                    rbb = sb(me, f"rbb{l}", [128, NEXP], F32)
                    bdn = sb(me, f"bdn{l}", [NEXP, D], F32)
                    bgu_r = sb(me, f"bgur{l}", [NEXP, 2 * D], F32)
                    bguT = sb(me, f"bguT{l}", [128, 16, NEXP], F32)
                    t_me = T("moeparams")
                    dma("sp", rw[:], prm["router_w"][l].rearrange("(k p) e -> p k e", p=128), w=[t_me])
                    dma("sp", rbb[:], prm["router_b"][l:l + 1, :].partition_broadcast(128), w=[t_me])
                    dma("sp", bdn[:], prm["exp_b_dn"][l], w=[t_me])
                    dma("sp", bgu_r[:], prm["exp_b_gu"][l], w=[t_me])
                    for j in range(16):
                        tr(ps[j % 8][:, 0:NEXP], bgu_r[:, j * 128:(j + 1) * 128], ident_f[0:NEXP, 0:NEXP], r=[t_me, t_const], w=[tps[j % 8]])
                        cp("dve", bguT[:, j, :], ps[j % 8][:, 0:NEXP], r=[tps[j % 8]], w=[t_me])
                    wgu = [sb(me, f"wgu{l}{i}", [128, 8, 2 * D], BF16) for i in range(2)]
                    t_wgu = [T(), T()]
                    wdn = sb(me, f"wdn{l}", [128, 8, D], BF16)
                    t_wdn = T()
                    h2T = sb(me, f"h2T{l}", [128, 8, 1024], BF16)
                    t_h2 = [T() for _ in range(8)]
                    acc = sb(me, f"acc{l}", [128, 8, D], F32)
                    t_acc = [T() for _ in range(8)]
                    gates = sb(me, f"gates{l}", [128, 8, NEXP], F32)
                    t_gt = [T() for _ in range(8)]
                    hid = [sb(me, f"hid{l}{i}", [128, 8, 512], BF16) for i in range(2)]
                    t_hid = [[T() for _ in range(8)] for _ in range(2)]
                    sw = [[sb(me, f"sw{l}{i}{j}", [128, 512], F32) for j in range(3)] for i in range(2)]
                    t_sw = [[T() for _ in range(3)] for _ in range(2)]
                    xm = [sb(me, f"xm{l}{i}", [128, D], F32) for i in range(2)]
                    t_xm = [T(), T()]
                    h2f = [sb(me, f"h2f{l}{i}", [128, 8, 128], F32) for i in range(2)]
                    t_h2f = [T(), T()]
                    junkm = sb(me, f"junkm{l}", [128, D], BF16)
                    t_junkm = T()
                    ssm = [sb(me, f"ssm{l}{i}", [128, 4], F32) for i in range(2)]
                    t_ssm = [T(), T()]
                    rt = [sb(me, f"rt{l}{i}", [128, 4, NEXP], F32) for i in range(2)]
                    t_rt = [T(), T()]
                    rs8 = [sb(me, f"rs8{l}{i}", [128, 12], F32) for i in range(2)]
                    gT = [sb(me, f"gT{l}{i}", [NEXP, 128], F32) for i in range(2)]
                    t_gT = [T(), T()]
                    g2b = sb(me, f"g2b{l}", [128, D], F32)
                    t_g2b = T()

                    groups = []
                    if not last:
                        tl = []
                        for s in seqs:
                            if s["ctx"]:
                                for ti in range(s["n"] // 128):
                                    r0 = s["off"] + ti * 128
                                    tl.append((r0, xb_d[r0:r0 + 128, :], t_xb))
                        groups.append((2, tl))
                    for s in seqs:
                        if s["ctx"]:
                            continue
                        for hf in range(2):
                            tl = []
                            for ti in range(8):
                                r0 = s["off"] + hf * 1024 + ti * 128
                                if last:
                                    dst = out_d[s["b"], hf * 1024 + ti * 128: hf * 1024 + (ti + 1) * 128, :]
                                    tl.append((r0, dst, t_out))
                                else:
                                    tl.append((r0, xb_d[r0:r0 + 128, :], t_xb))
                            groups.append((s["mv"], tl))

                    ecount = 0
                    tcount = 0
                    for (mv, tl) in groups:
                        ntl = len(tl)
                        nsub = ntl // 4
                        dma("sp", g2b[:], mod_d[l, mv:mv + 1, 5 * D:6 * D].partition_broadcast(128), r=[t_mod], w=[t_g2b])
                        for ti, (r0, dst, tdst) in enumerate(tl):
                            par = tcount % 2
                            tcount += 1
                            xtt, t_x = xm[par], t_xm[par]
                            dma("sp", xtt[:], xa_d[r0:r0 + 128, :], r=[t_xa], w=[t_x])
                            rstd = rms_rstd((junkm, t_junkm, ssm[par], t_ssm[par]), xtt[:], t_x, D, None)
                            ts("dve", xtt[:], xtt[:], rstd, None, ALU.mult, r=[t_x, t_ssm[par]], w=[t_x])
                            pA, tpA = ps[2 * par], tps[2 * par]
                            pB, tpB = ps[2 * par + 1], tps[2 * par + 1]
                            for k in range(8):
                                pp, tpp = (pA, tpA) if k < 4 else (pB, tpB)
                                tr(pp[:, (k % 4) * 128:(k % 4 + 1) * 128], xtt[:, k * 128:(k + 1) * 128], ident_f[:], r=[t_x, t_const], w=[tpp])
                            hf_, thf = h2f[par], t_h2f[par]
                            for k in range(8):
                                pp, tpp = (pA, tpA) if k < 4 else (pB, tpB)
                                i_ = pp[:, (k % 4) * 128:(k % 4 + 1) * 128]
                                if k % 2 == 0:
                                    act(hf_[:, k, :], i_, AF.Identity, r=[tpp, t_lp], w=[thf], bias=AB[:, 3, mv, k:k + 1], scale=AB[:, 2, mv, k:k + 1])
                                else:
                                    ts("dve", hf_[:, k, :], i_, AB[:, 2, mv, k:k + 1], AB[:, 3, mv, k:k + 1], ALU.mult, ALU.add, r=[tpp, t_lp], w=[thf])
                            cp("pool", h2T[:, :, ti * 128:(ti + 1) * 128], hf_[:, :, :], r=[thf], w=[t_h2[ti]])
                            pr, tpr = ps[4 + par], tps[4 + par]
                            for k in range(8):
                                mm(pr[:, 0:NEXP], hf_[:, k, :], rw[:, k, :], k == 0, k == 7, r=[thf, t_me], w=[tpr])
                            R_, tR = rt[par], t_rt[par]
                            s8 = rs8[par]
                            tt("dve", R_[:, 0, :], pr[:, 0:NEXP], rbb[:], ALU.add, r=[tpr, t_me], w=[tR])
                            P.op("dve", (lambda o, i_: (lambda e: e.max(out=o, in_=i_)))(s8[:, 0:8], R_[:, 0, :]), [tR], [tR])
                            ts("dve", R_[:, 1, :], R_[:, 0, :], s8[:, 3:4], None, ALU.is_ge, r=[tR], w=[tR])
                            ts("dve", s8[:, 8:9], s8[:, 0:1], -1.0, None, ALU.mult, r=[tR], w=[tR])
                            act(R_[:, 2, :], R_[:, 0, :], AF.Exp, r=[tR], w=[tR], bias=s8[:, 8:9], scale=1.0)
                            tt("dve", R_[:, 2, :], R_[:, 2, :], R_[:, 1, :], ALU.mult, r=[tR], w=[tR])
                            act(R_[:, 3, :], R_[:, 2, :], AF.Identity, r=[tR], w=[tR], accum=s8[:, 9:10])
                            rcp(s8[:, 10:11], s8[:, 9:10], r=[tR], w=[tR])
                            ts("dve", gates[:, ti, :], R_[:, 2, :], s8[:, 10:11], None, ALU.mult, r=[tR], w=[t_gt[ti]])
                            pg, tpg = ps[6], tps[6]
                            tr(pg[0:NEXP, 0:128], gates[:, ti, :], ident_f[:], r=[t_gt[ti], t_const], w=[tpg])
                            cp("act", gT[par][:], pg[0:NEXP, 0:128], r=[tpg], w=[t_gT[par]])
                            for dh in range(2):
                                pb, tpb = ps[7], tps[7]
                                mm(pb[:, :], gT[par][:], bdn[:, dh * 512:(dh + 1) * 512], True, True, r=[t_gT[par], t_me], w=[tpb])
                                cp("act", acc[:, ti, dh * 512:(dh + 1) * 512], pb[:, :], r=[tpb], w=[t_acc[ti]])
                        gring = Ring([0, 1, 2, 3])
                        dring = Ring([4, 5, 6, 7])
                        for ex in range(NEXP):
                            wb, twb = wgu[ecount % 2], t_wgu[ecount % 2]
                            ecount += 1
                            dma("pool", wdn[:], prm["exp_w_dn"][l, ex].rearrange("(k p) f -> p k f", p=128), w=[t_wdn])
                            dma("pool", wb[:], prm["exp_w_gu"][l, ex].rearrange("(k p) f -> p k f", p=128), w=[twb])
                            for sgi in range(nsub):
                                hb, thb = hid[sgi % 2], t_hid[sgi % 2]
                                rh = [t_h2[sgi * 4 + q_] for q_ in range(4)]
                                for j in range(8):
                                    pgl, tpgl = gring.next()
                                    pup, tpup = gring.next()
                                    for k in range(8):
                                        mm(pgl[:, :], wb[:, k, j * 128:(j + 1) * 128], h2T[:, k, sgi * 512:(sgi + 1) * 512], k == 0, k == 7,
                                           r=[twb] + rh, w=[tpgl])
                                    for k in range(8):
                                        mm(pup[:, :], wb[:, k, D + j * 128: D + (j + 1) * 128], h2T[:, k, sgi * 512:(sgi + 1) * 512], k == 0, k == 7,
                                           r=[twb] + rh, w=[tpup])
                                    sp_ = j % 2
                                    a_, s_, u_ = sw[sp_]
                                    ta_, ts_, tu_ = t_sw[sp_]
                                    ts("dve", a_[:], pgl[:, :], bguT[:, j, ex:ex + 1], 7.0, ALU.add, ALU.min, r=[tpgl, t_me], w=[ta_])
                                    act(s_[:], a_[:], AF.Sigmoid, r=[ta_], w=[ts_], scale=1.702)
                                    act(u_[:], pup[:, :], AF.Identity, r=[tpup, t_me], w=[tu_], bias=bguT[:, 8 + j, ex:ex + 1])
                                    ts("dve", u_[:], u_[:], 7.0, -7.0, ALU.min, ALU.max, r=[tu_], w=[tu_])
                                    tt("pool", a_[:], a_[:], s_[:], ALU.mult, r=[ta_, ts_], w=[ta_])
                                    stt(hb[:, j, :], u_[:], 1.0, a_[:], ALU.add, ALU.mult, r=[ta_, tu_], w=[thb[j]])
                                for q_ in range(4):
                                    ti = sgi * 4 + q_
                                    for dh in range(2):
                                        pd, tpd = dring.next()
                                        for j in range(8):
                                            mm(pd[:, :], hb[:, j, q_ * 128:(q_ + 1) * 128], wdn[:, j, dh * 512:(dh + 1) * 512], j == 0, j == 7,
                                               r=[thb[j], t_wdn], w=[tpd])
                                        stt(acc[:, ti, dh * 512:(dh + 1) * 512], pd[:, :], gates[:, ti, ex:ex + 1], acc[:, ti, dh * 512:(dh + 1) * 512],
                                            ALU.mult, ALU.add, r=[tpd, t_gt[ti], t_acc[ti]], w=[t_acc[ti]])
                        for ti, (r0, dst, tdst) in enumerate(tl):
                            par = tcount % 2
                            tcount += 1
                            xtt, t_x = xm[par], t_xm[par]
                            dma("sp", xtt[:], xa_d[r0:r0 + 128, :], r=[t_xa], w=[t_x])
                            tt("dve", acc[:, ti, :], acc[:, ti, :], g2b[:], ALU.mult, r=[t_acc[ti], t_g2b], w=[t_acc[ti]])
                            tt("pool", xtt[:], xtt[:], acc[:, ti, :], ALU.add, r=[t_x, t_acc[ti]], w=[t_x])
                            dma("sp", dst, xtt[:], r=[t_x], w=[tdst])
                    P.barrier()
                if dbg and "dbg_xb" in dbg_d and l == 0:
                    dma("sp", dbg_d["dbg_xb"], xb_d, r=[t_xb], w=[T("dbg3")])
        try:
            _layers()
        except _Stop:
            pass
        P.barrier()
        P.emit()
    return nc


def _consts():
    ident = np.eye(128, dtype=np.float32)
    blk = np.zeros((64, 64), np.float32)
    blk[0:32, 0:32] = 1.0
    blk[32:64, 32:64] = 1.0
    perm = np.zeros((64, 64), np.float32)
    for i in range(64):
        d = i % 32
        base = i - d
        half = (d // 16) * 16
        dd = d % 16
        partner = base + half + ((dd + 8) % 16)
        perm[partner, i] = 1.0
    rows = SEQ // 64
    row = np.repeat(np.arange(rows, dtype=np.float32), 64)
    col = np.tile(np.arange(64, dtype=np.float32), rows)
    nf = 8
    inv = (np.float32(10000.0) ** (-np.arange(nf, dtype=np.float32) / np.float32(nf))).astype(np.float32)
    ar = (row[:, None] * inv).astype(np.float32)
    ac = (col[:, None] * inv).astype(np.float32)
    C = np.zeros((64, SEQ), np.float32)
    S = np.zeros((64, SEQ), np.float32)
    for i in range(64):
        d = i % 32
        ang = ar if d < 16 else ac
        dd = d % 16
        f = dd % 8
        C[i] = np.cos(ang[:, f])
        S[i] = (-np.sin(ang[:, f])) if dd < 8 else np.sin(ang[:, f])
    ltri = np.triu(np.ones((128, 128), np.float32), k=1)
    eiota = np.arange(128, dtype=np.float32).reshape(128, 1)
    pk = np.zeros((128, 16), np.float32)
    for l in range(DEPTH):
        for k in range(8):
            pk[:, l * 8 + k] = l * NEXP * 1024 + k * 128 + np.arange(128)
    return dict(k_ident=ident, k_blk=blk, k_perm=perm, k_ropec=C, k_ropes=S, k_ltri=ltri, k_eiota=eiota, k_pk=pk)


_NC_CACHE = {}


def _make_in_maps(inputs):
    f32 = lambda a: np.ascontiguousarray(np.asarray(a, dtype=np.float32))
    consts = _consts()
    shared = {}
    for k, shp in PARAM_SHAPES.items():
        shared[k] = f32(inputs[k]).reshape(shp)
    x = f32(inputs["x"]); ctx = f32(inputs["ctx"]); c = f32(inputs["c"]); c_ctx = f32(inputs["c_ctx"]).reshape(1, D)
    in_maps = []
    for i in range(NCORES):
        m = dict(shared)
        m.update(consts)
        m["x"] = x[i * NB:(i + 1) * NB]
        m["ctx"] = ctx[i * NB:(i + 1) * NB]
        m["c"] = np.ascontiguousarray(np.concatenate([c[i * NB:(i + 1) * NB], c_ctx], axis=0))
        in_maps.append(m)
    return in_maps


def kernel(**inputs):
    if "nc" not in _NC_CACHE:
        _NC_CACHE["nc"] = build_nc()
    nc = _NC_CACHE["nc"]
    in_maps = _make_in_maps(inputs)
    res = run_bass_kernel_spmd(nc, in_maps, core_ids=list(range(NCORES)))
    out = np.concatenate([np.asarray(r["out"], dtype=np.float32) for r in res.results], axis=0)
    return out
```

```python
import math
from contextlib import ExitStack
import numpy as np
import concourse.bass as bass
import concourse.mybir as mybir
from concourse.bass_utils import run_bass_kernel_spmd
from concourse.bass import IndirectOffsetOnAxis

F32 = mybir.dt.float32
BF16 = mybir.dt.bfloat16
ALU = mybir.AluOpType
AF = mybir.ActivationFunctionType

NCORES = 8
DEPTH = 2
D = 1024
SEQ = 2048
CTX = 256
NB = 2
EPS = 1e-6
NEXP = 32
MOE_SPARSE = True
U32 = mybir.dt.uint32
ENGS = ("pe", "act", "dve", "pool", "sp")


class T:
    __slots__ = ("name", "w", "r")

    def __init__(self, name=""):
        self.name = name
        self.w = None
        self.r = []


class Prog:
    def __init__(self, nc, stack, n_dma_sems=8):
        self.nc = nc
        self.q = {e: [] for e in ENGS}
        self.cnt = {e: 0 for e in ENGS}
        self.sem = {e: stack.enter_context(nc.semaphore("s_" + e)) for e in ENGS}
        self.seen = {e: {} for e in ENGS}
        self.dsem, self.dval, self.dnext = {}, {}, {}
        for qn in ("sp", "act", "pool"):
            self.dsem[qn] = [stack.enter_context(nc.semaphore(f"d_{qn}{i}")) for i in range(n_dma_sems)]
            self.dval[qn] = [0] * n_dma_sems
            self.dnext[qn] = 0
        self.pending = {e: False for e in ENGS}

    def _need(self, eng, ev, waits):
        if ev is None:
            return
        key = ev[0:2]
        if eng == "pe" and key == ("eng", "pe"):
            return
        if self.seen[eng].get(key, 0) >= ev[2]:
            return
        if waits.get(key, 0) < ev[2]:
            waits[key] = ev[2]

    def _deps(self, eng, reads, writes):
        waits = {}
        for t in reads:
            self._need(eng, t.w, waits)
        for t in writes:
            self._need(eng, t.w, waits)
            for ev in t.r:
                self._need(eng, ev, waits)
        for key, v in waits.items():
            self.seen[eng][key] = v
        return list(waits.items())

    def _mark(self, ev, reads, writes):
        for t in reads:
            t.r.append(ev)
            if len(t.r) > 48:
                best = {}
                for e in t.r:
                    k = e[0:2]
                    if best.get(k, 0) < e[2]:
                        best[k] = e[2]
                t.r = [(k[0], k[1], v) for k, v in best.items()]
        for t in writes:
            t.w = ev
            t.r = []

    def op(self, eng, fn, reads=(), writes=(), inc=True):
        waits = self._deps(eng, reads, writes)
        ev = ("eng", eng, self.cnt[eng] + 1)
        if inc:
            self.cnt[eng] += 1
            self.pending[eng] = False
        else:
            self.pending[eng] = True
        self.q[eng].append((waits, fn, inc, None))
        self._mark(ev, reads, writes)

    def dma(self, qn, fn, reads=(), writes=()):
        waits = self._deps(qn, reads, writes)
        i = self.dnext[qn]
        self.dnext[qn] = (i + 1) % len(self.dsem[qn])
        if self.dval[qn][i] > 0:
            key = ("dma", (qn, i))
            if self.seen[qn].get(key, 0) < self.dval[qn][i]:
                waits.append((key, self.dval[qn][i]))
                self.seen[qn][key] = self.dval[qn][i]
        self.dval[qn][i] += 16
        ev = ("dma", (qn, i), self.dval[qn][i])
        self.q[qn].append((waits, fn, False, (qn, i)))
        self._mark(ev, reads, writes)

    def barrier(self):
        evs = [("eng", e, self.cnt[e]) for e in ENGS if self.cnt[e] > 0]
        for qn in self.dsem:
            for i, v in enumerate(self.dval[qn]):
                if v > 0:
                    evs.append(("dma", (qn, i), v))
        for e in ENGS:
            assert not self.pending[e]
            waits = {}
            for ev in evs:
                self._need(e, ev, waits)
            for key, v in waits.items():
                self.seen[e][key] = v
            self.q[e].append((list(waits.items()), None, False, None))

    def check(self):
        val = {}
        pos = {e: 0 for e in ENGS}
        progress = True
        while progress:
            progress = False
            for e in ENGS:
                q = self.q[e]
                while pos[e] < len(q):
                    waits, fn, inc, d = q[pos[e]]
                    if any(val.get(k, 0) < v for k, v in waits):
                        break
                    if fn is not None:
                        if d is not None:
                            val[("dma", d)] = val.get(("dma", d), 0) + 16
                        elif inc:
                            val[("eng", e)] = val.get(("eng", e), 0) + 1
                    pos[e] += 1
                    progress = True
        stuck = {e: (pos[e], len(self.q[e])) for e in ENGS if pos[e] < len(self.q[e])}
        if stuck:
            msg = []
            for e, (p, n) in stuck.items():
                waits = self.q[e][p][0]
                msg.append(f"{e} stuck at {p}/{n} waits={[(k, v, val.get(k, 0)) for k, v in waits if val.get(k, 0) < v]}")
            raise RuntimeError("deadlock: " + "; ".join(msg))

    def _semof(self, key):
        if key[0] == "eng":
            return self.sem[key[1]]
        qn, i = key[1]
        return self.dsem[qn][i]

    def emit(self):
        nc = self.nc
        for e in ENGS:
            assert not self.pending[e], e
        self.check()
        with nc.Block() as block:
            def run(engname):
                def body(eng):
                    for waits, fn, inc, d in self.q[engname]:
                        for key, v in waits:
                            eng.wait_ge(self._semof(key), v)
                        if fn is None:
                            continue
                        ins = fn(eng)
                        if d is not None:
                            ins.then_inc(self.dsem[d[0]][d[1]], 16)
                        elif inc:
                            ins.then_inc(self.sem[engname], 1)
                return body
            block.tensor(run("pe"))
            block.scalar(run("act"))
            block.vector(run("dve"))
            block.gpsimd(run("pool"))
            block.sync(run("sp"))


PARAM_SHAPES = {
    "w_mod": [DEPTH, D, 6 * D], "b_mod": [DEPTH, 6 * D], "norm1_g": [DEPTH, D], "norm2_g": [DEPTH, D],
    "w_in": [DEPTH, D, 2560], "a_vnorm_g": [DEPTH, 256], "a_ws": [DEPTH, 4, 128, 128], "a_bs": [DEPTH, 512],
    "b_qnorm_g": [DEPTH, 64], "b_knorm_g": [DEPTH, 64], "b_lam_q1": [DEPTH, 32], "b_lam_k1": [DEPTH, 32],
    "b_lam_q2": [DEPTH, 32], "b_lam_k2": [DEPTH, 32], "b_subln_g": [DEPTH, 64],
    "c_conv_w": [DEPTH, 31, 256], "c_conv_b": [DEPTH, 256], "c_ln_g": [DEPTH, 256], "c_ln_b": [DEPTH, 256],
    "d_conv_w": [DEPTH, 3, 256], "w_out": [DEPTH, D, D], "router_w": [DEPTH, D, NEXP], "router_b": [DEPTH, NEXP],
    "exp_w_gu": [DEPTH, NEXP, D, 2 * D], "exp_b_gu": [DEPTH, NEXP, 2 * D], "exp_w_dn": [DEPTH, NEXP, D, D],
    "exp_b_dn": [DEPTH, NEXP, D],
}


class _Stop(Exception):
    pass


def build_nc(n_layers=DEPTH, dbg=None, skip_moe=False, stop=None):
    nc = bass.Bass("TRN2", target_bir_lowering=False)
    din = lambda n, s: nc.dram_tensor(n, s, F32, kind="ExternalInput").ap()
    x_in = din("x", [NB, SEQ, D])
    ctx_in = din("ctx", [NB, CTX, D])
    c_in = din("c", [3, D])
    prm = {k: din(k, s) for k, s in PARAM_SHAPES.items()}
    ident_in = din("k_ident", [128, 128])
    blk_in = din("k_blk", [64, 64])
    perm_in = din("k_perm", [64, 64])
    ropec_in = din("k_ropec", [64, SEQ])
    ropes_in = din("k_ropes", [64, SEQ])
    ltri_in = din("k_ltri", [128, 128])
    eiota_in = din("k_eiota", [128, 1])
    pk_in = din("k_pk", [128, 16])
    out_d = nc.dram_tensor("out", [NB, SEQ, D], F32, kind="ExternalOutput").ap()
    NTOK = NB * (SEQ + CTX)
    xa_d = nc.dram_tensor("xa_scr", [NTOK, D], F32, kind="Internal").ap()
    xb_d = nc.dram_tensor("xb_scr", [NTOK, D], F32, kind="Internal").ap()
    mod_d = nc.dram_tensor("mod_scr", [DEPTH, 3, 6 * D], F32, kind="Internal").ap()
    NSLOT = (NTOK * 4 // 512 + NEXP) * 512
    h2_d = nc.dram_tensor("h2_scr", [NTOK, D], F32, kind="Internal").ap()
    xp_d = nc.dram_tensor("xp_scr", [NSLOT, D], F32, kind="Internal").ap()
    y_d = nc.dram_tensor("y_scr", [NSLOT, D], F32, kind="Internal").ap()
    wgu_flat = prm["exp_w_gu"].rearrange("l e k f -> (l e k) f")
    wdn_flat = prm["exp_w_dn"].rearrange("l e k f -> (l e k) f")
    dbg_d = {}
    if dbg:
        for name, shape in dbg.items():
            dbg_d[name] = nc.dram_tensor(name, shape, F32, kind="ExternalOutput").ap()

    seqs = []
    off = 0
    for b in range(NB):
        seqs.append(dict(ctx=True, b=b, n=CTX, off=off, mv=2)); off += CTX
        seqs.append(dict(ctx=False, b=b, n=SEQ, off=off, mv=b)); off += SEQ
    t_xa, t_xb, t_mod, t_out = T("xa"), T("xb"), T("mod"), T("out")

    def seq_src(s, l):
        if l == 0:
            return (ctx_in[s["b"]] if s["ctx"] else x_in[s["b"]]), None
        return xb_d[s["off"]:s["off"] + s["n"], :], t_xb

    with ExitStack() as st:
        P = Prog(nc, st)
        ncd = nc.allow_non_contiguous_dma(reason="small parameter layout loads")
        st.enter_context(ncd)

        def mm(out, lhsT, rhs, start, stop, r=(), w=()):
            P.op("pe", lambda e: e.matmul(out, lhsT=lhsT, rhs=rhs, start=start, stop=stop), r, w, inc=stop)

        def tr(out, in_, ident, r=(), w=()):
            P.op("pe", lambda e: e.transpose(out, in_, ident), r, w)

        def act(out, in_, func, r=(), w=(), bias=0.0, scale=1.0, accum=None):
            if accum is None:
                P.op("act", lambda e: e.activation(out, in_, func, bias=bias, scale=scale), r, w)
            else:
                P.op("act", lambda e: e.activation(out, in_, func, bias=bias, scale=scale, accum_out=accum), r, w)

        def tt(eng, out, a, b, op, r=(), w=()):
            P.op(eng, lambda e: e.tensor_tensor(out, a, b, op), r, w)

        def ts(eng, out, a, s1, s2, op0, op1=None, r=(), w=()):
            if op1 is None:
                P.op(eng, lambda e: e.tensor_scalar(out, a, s1, None, op0), r, w)
            else:
                P.op(eng, lambda e: e.tensor_scalar(out, a, s1, s2, op0, op1), r, w)

        def stt(out, in0, scalar, in1, op0, op1, r=(), w=()):
            P.op("dve", lambda e: e.scalar_tensor_tensor(out, in0, scalar, in1, op0, op1), r, w)

        def cp(eng, out, in_, r=(), w=()):
            if eng == "act":
                act(out, in_, AF.Identity, r, w)
            else:
                P.op(eng, lambda e: e.tensor_copy(out, in_), r, w)

        def rcp(out, in_, r=(), w=()):
            P.op("dve", lambda e: e.reciprocal(out, in_), r, w)

        def mset(eng, ap, val, w=()):
            P.op(eng, lambda e: e.memset(ap, val), (), w)

        def dma(qn, out, in_, r=(), w=()):
            P.dma(qn, lambda e: e.dma_start(out=out, in_=in_), r, w)

        _uid = [0]

        def sb(scope, name, shape, dt):
            _uid[0] += 1
            return scope.enter_context(nc.sbuf_tensor(f"{name}_u{_uid[0]}", shape, dt))

        ps = [st.enter_context(nc.psum_tensor(f"ps{i}", [128, 512], F32)) for i in range(8)]
        tps = [T(f"ps{i}") for i in range(8)]

        class Ring:
            def __init__(self, idxs):
                self.idxs = idxs
                self.i = 0

            def next(self):
                k = self.idxs[self.i % len(self.idxs)]
                self.i += 1
                return ps[k], tps[k]

        _bp = [0]

        def bp_hit():
            _bp[0] += 1
            return stop == f"bp{_bp[0]}"

        ident_f = sb(st, "ident_f", [128, 128], F32)
        ident_b = sb(st, "ident_b", [128, 128], BF16)
        blk64 = sb(st, "blk64", [64, 64], F32)
        perm64 = sb(st, "perm64", [64, 64], F32)
        ones256 = sb(st, "ones256", [128, 128], F32)
        t_const = T("const")
        dma("sp", ident_f[:], ident_in, w=[t_const])
        dma("sp", blk64[:], blk_in, w=[t_const])
        dma("sp", perm64[:], perm_in, w=[t_const])
        t_identb = T("identb")
        cp("dve", ident_b[:], ident_f[:], r=[t_const], w=[t_identb])
        t_ones = T("ones")
        mset("pool", ones256[:], 1.0 / 256.0, w=[t_ones])

        def rms_rstd(scope_tiles, x_ap, t_x, ncols, tagw):
            junk, t_junk, ss, t_ss = scope_tiles
            act(junk[:, 0:ncols], x_ap, AF.Square, r=[t_x], w=[t_junk, t_ss], accum=ss[:, 0:1])
            act(ss[:, 1:2], ss[:, 0:1], AF.Sqrt, r=[t_ss], w=[t_ss], bias=EPS, scale=1.0 / ncols)
            rcp(ss[:, 2:3], ss[:, 1:2], r=[t_ss], w=[t_ss])
            return ss[:, 2:3]

        def _layers():
          for l in range(n_layers):
            last = (l == DEPTH - 1)
            lam_init = 0.8 - 0.6 * math.exp(-0.3 * l)
            with ExitStack() as ls:
                n1g = sb(ls, f"n1g{l}", [128, 8], F32)
                n2g = sb(ls, f"n2g{l}", [128, 8], F32)
                modP = sb(ls, f"modP{l}", [128, 4, 3, 8], F32)
                AB = sb(ls, f"AB{l}", [128, 4, 3, 8], F32)
                t_lp = T("layerparams")
                dma("sp", n1g[:], prm["norm1_g"][l].rearrange("(k p) -> p k", p=128), w=[t_lp])
                dma("sp", n2g[:], prm["norm2_g"][l].rearrange("(k p) -> p k", p=128), w=[t_lp])

                with ExitStack() as ms:
                    cT = sb(ms, f"cT{l}", [128, 8, 3], F32)
                    scT = sb(ms, f"scT{l}", [128, 8, 3], BF16)
                    bmod = sb(ms, f"bmod{l}", [3, 6 * D], F32)
                    modrow = sb(ms, f"modrow{l}", [3, 6 * D], F32)
                    wms = [sb(ms, f"wms{l}_{i}", [128, 8, 512], BF16) for i in range(2)]
                    t_wms = [T("wms0"), T("wms1")]
                    t_c, t_bm, t_mr = T("c"), T("bmod"), T("modrow")
                    for v in range(3):
                        dma("sp", cT[:, :, v], c_in[v].rearrange("(k p) -> p k", p=128), w=[t_c])
                    dma("sp", bmod[:], prm["b_mod"][l:l + 1, :].partition_broadcast(3), w=[t_bm])
                    act(scT[:], cT[:], AF.Silu, r=[t_c], w=[t_c])
                    for n in range(12):
                        wb, twb = wms[n % 2], t_wms[n % 2]
                        dma("pool", wb[:], prm["w_mod"][l][:, n * 512:(n + 1) * 512].rearrange("(k p) f -> p k f", p=128), w=[twb])
                        pb, tpb = ps[n % 2], tps[n % 2]
                        for k in range(8):
                            mm(pb[0:3, :], scT[:, k, :], wb[:, k, :], k == 0, k == 7, r=[t_c, twb], w=[tpb])
                        tt("dve", modrow[:, n * 512:(n + 1) * 512], pb[0:3, :], bmod[:, n * 512:(n + 1) * 512], ALU.add,
                           r=[tpb, t_bm], w=[t_mr])
                    dma("sp", mod_d[l], modrow[:], r=[t_mr], w=[t_mod])
                    for si, sec in enumerate((0, 1, 3, 4)):
                        for v in range(3):
                            dma("sp", modP[:, si, v, :], mod_d[l, v, sec * D:(sec + 1) * D].rearrange("(k p) -> p k", p=128),
                                r=[t_mod], w=[t_lp])
                    for v in range(3):
                        stt(AB[:, 0, v, :], modP[:, 1, v, :], 1.0, n1g[:], ALU.add, ALU.mult, r=[t_lp], w=[t_lp])
                        cp("dve", AB[:, 1, v, :], modP[:, 0, v, :], r=[t_lp], w=[t_lp])
                        stt(AB[:, 2, v, :], modP[:, 3, v, :], 1.0, n2g[:], ALU.add, ALU.mult, r=[t_lp], w=[t_lp])
                        cp("dve", AB[:, 3, v, :], modP[:, 2, v, :], r=[t_lp], w=[t_lp])
                    P.barrier()
                if stop == "mod":
                    dma("sp", dbg_d["dbg_mod"], mod_d[0], r=[t_mod], w=[T("dbg2")])
                    return

                with ExitStack() as mx:
                    vngb = sb(mx, f"vngb{l}", [128, 256], F32)
                    bsb = sb(mx, f"bsb{l}", [128, 512], F32)
                    wsT = sb(mx, f"wsT{l}", [128, 4, 128], BF16)
                    gqk = sb(mx, f"gqk{l}", [64, 2], F32)
                    lamv = sb(mx, f"lamv{l}", [128, 4, 32], F32)
                    lams = sb(mx, f"lams{l}", [128, 8], F32)
                    sublnb = sb(mx, f"sublnb{l}", [128, 64], F32)
                    prow = sb(mx, f"prow{l}", [37, 256], F32)
                    pcol = sb(mx, f"pcol{l}", [128, 2, 37], F32)
                    diag = sb(mx, f"diag{l}", [128, 2, 31, 128], BF16)
                    ropec = sb(mx, f"ropec{l}", [64, SEQ], F32)
                    ropes = sb(mx, f"ropes{l}", [64, SEQ], F32)
                    t_mp = T("mixparams")
                    t_diag = T("diag")
                    dma("sp", ropec[:], ropec_in, w=[t_mp])
                    dma("sp", ropes[:], ropes_in, w=[t_mp])
                    dma("sp", vngb[:], prm["a_vnorm_g"][l:l + 1, :].partition_broadcast(128), w=[t_mp])
                    dma("sp", bsb[:], prm["a_bs"][l:l + 1, :].partition_broadcast(128), w=[t_mp])
                    dma("sp", gqk[:, 0:1], prm["b_qnorm_g"][l].rearrange("(p o) -> p o", o=1), w=[t_mp])
                    dma("sp", gqk[:, 1:2], prm["b_knorm_g"][l].rearrange("(p o) -> p o", o=1), w=[t_mp])
                    for i, nm in enumerate(("b_lam_q1", "b_lam_k1", "b_lam_q2", "b_lam_k2")):
                        dma("sp", lamv[:, i, :], prm[nm][l:l + 1, :].partition_broadcast(128), w=[t_mp])
                    dma("sp", sublnb[:], prm["b_subln_g"][l:l + 1, :].partition_broadcast(128), w=[t_mp])
                    dma("sp", prow[0:31, :], prm["c_conv_w"][l], w=[t_mp])
                    dma("sp", prow[31:32, :], prm["c_conv_b"][l:l + 1, :], w=[t_mp])
                    dma("sp", prow[32:33, :], prm["c_ln_g"][l:l + 1, :], w=[t_mp])
                    dma("sp", prow[33:34, :], prm["c_ln_b"][l:l + 1, :], w=[t_mp])
                    dma("sp", prow[34:37, :], prm["d_conv_w"][l], w=[t_mp])
                    ts("dve", gqk[:, 0:1], gqk[:, 0:1], 32.0 ** -0.5, None, ALU.mult, r=[t_mp], w=[t_mp])
                    tt("dve", lamv[:, 0, :], lamv[:, 0, :], lamv[:, 1, :], ALU.mult, r=[t_mp], w=[t_mp])
                    tt("dve", lamv[:, 2, :], lamv[:, 2, :], lamv[:, 3, :], ALU.mult, r=[t_mp], w=[t_mp])
                    act(lamv[:, 1, :], lamv[:, 0, :], AF.Identity, r=[t_mp], w=[t_mp], accum=lams[:, 0:1])
                    act(lamv[:, 3, :], lamv[:, 2, :], AF.Identity, r=[t_mp], w=[t_mp], accum=lams[:, 1:2])
                    act(lams[:, 2:4], lams[:, 0:2], AF.Exp, r=[t_mp], w=[t_mp])
                    tt("dve", lams[:, 4:5], lams[:, 3:4], lams[:, 2:3], ALU.subtract, r=[t_mp], w=[t_mp])
                    ts("dve", lams[:, 4:5], lams[:, 4:5], -lam_init, None, ALU.add, r=[t_mp], w=[t_mp])
                    ts("dve", sublnb[:], sublnb[:], 1.0 - lam_init, None, ALU.mult, r=[t_mp], w=[t_mp])
                    with ExitStack() as tmp:
                        wsf = sb(tmp, f"wsf{l}", [128, 4, 128], F32)
                        t_wsf = T("wsf")
                        for h in range(4):
                            dma("sp", wsf[:, h, :], prm["a_ws"][l, h], w=[t_wsf])
                        for h in range(4):
                            tr(ps[h][:, 0:128], wsf[:, h, :], ident_f[:], r=[t_wsf, t_const], w=[tps[h]])
                            cp("dve", wsT[:, h, :], ps[h][:, 0:128], r=[tps[h]], w=[t_mp])
                        for c in range(2):
                            tr(ps[4 + c][:, 0:37], prow[:, c * 128:(c + 1) * 128], ident_f[0:37, 0:37], r=[t_mp, t_const], w=[tps[4 + c]])
                            cp("dve", pcol[:, c, :], ps[4 + c][:, 0:37], r=[tps[4 + c]], w=[t_mp])
                        P.barrier()
                    for c in range(2):
                        for k in range(31):
                            ts("pool" if (k % 2) else "dve", diag[:, c, k, :], ident_b[:], pcol[:, c, k:k + 1], None, ALU.mult,
                               r=[t_identb, t_mp], w=[t_diag])
                    if stop == "params":
                        return

                    for b in range(NB):
                        with ExitStack() as bs:
                            qT = sb(bs, f"qT{l}{b}", [64, 4, SEQ], BF16)
                            kT = sb(bs, f"kT{l}{b}", [64, 4, SEQ + CTX], BF16)
                            Vaug = sb(bs, f"Va{l}{b}", [128, 18, 4, 65], BF16)
                            zbuf = sb(bs, f"zb{l}{b}", [128, 2, SEQ + 30], BF16)
                            ebuf = sb(bs, f"eb{l}{b}", [128, 2, SEQ + 2], BF16)
                            yT = sb(bs, f"yT{l}{b}", [128, 8, SEQ], BF16)
                            t_q = [[T() for _ in range(4)] for _ in range(4)]
                            t_k = [[T() for _ in range(5)] for _ in range(4)]
                            t_v = [T() for _ in range(18)]
                            t_z = [[T() for _ in range(5)] for _ in range(2)]
                            t_e = [[T() for _ in range(5)] for _ in range(2)]
                            t_y = [[T() for _ in range(4)] for _ in range(8)]
                            mset("pool", Vaug[:, :, :, 64:65], 1.0, w=t_v)
                            for sq_ in (seqs[2 * b], seqs[2 * b + 1]):
                                n = sq_["n"]
                                isctx = sq_["ctx"]
                                mv = sq_["mv"]
                                src, t_src = seq_src(sq_, l)
                                rsrc = [t_src] if t_src is not None else []
                                kv_only = isctx and last
                                ngr = max(1, n // 512)
                                N = min(n, 512)
                                ntile = N // 128
                                koff = SEQ if isctx else 0
                                ktile0 = 16 if isctx else 0
                                if not kv_only:
                                    for c in range(2):
                                        mset("pool", zbuf[:, c, 0:15], 0.0, w=[t_z[c][4]])
                                        mset("pool", zbuf[:, c, 15 + n:30 + n], 0.0, w=[t_z[c][4]])
                                        mset("pool", ebuf[:, c, 0:1], 0.0, w=[t_e[c][4]])
                                        mset("pool", ebuf[:, c, 1 + n:2 + n], 0.0, w=[t_e[c][4]])
                                for pas in range(1 if kv_only else 2):
                                    with ExitStack() as p1:
                                        win = sb(p1, f"win{l}{b}{int(isctx)}{pas}", [128, 8, 1280], BF16)
                                        t_win = T("win")
                                        dma("pool", win[:], prm["w_in"][l][:, pas * 1280:(pas + 1) * 1280].rearrange("(k p) f -> p k f", p=128),
                                            w=[t_win])
                                        hT = [sb(p1, f"hT{i}", [128, 8, 512], BF16) for i in range(2)]
                                        t_hT = [T(), T()]
                                        xt = [sb(p1, f"xt{i}", [128, D], F32) for i in range(2)]
                                        t_xt = [T(), T()]
                                        junk = sb(p1, "junk", [128, D], BF16)
                                        t_junk = T()
                                        ssn = [sb(p1, f"ssn{i}", [128, 4], F32) for i in range(2)]
                                        t_ssn = [T(), T()]
                                        tmpf = [sb(p1, f"tmpf{i}", [128, 512], F32) for i in range(8)]
                                        t_tmpf = [T() for _ in range(8)]
                                        uT = [sb(p1, f"uT{i}", [128, 2, 512], F32) for i in range(2)]
                                        t_uT = [[T(), T()], [T(), T()]]
                                        vn = [sb(p1, f"vn{i}", [128, 256], BF16) for i in range(2)]
                                        t_vn = [T(), T()]
                                        ssv = [sb(p1, f"ssv{i}", [128, 12], F32) for i in range(2)]
                                        t_ssv = [T(), T()]
                                        ring = Ring([4, 5, 6, 7])
                                        tcount = 0
                                        tmpi = 0
                                        for g in range(ngr):
                                            hTg, t_hTg = hT[g % 2], t_hT[g % 2]
                                            tok0 = g * 512
                                            for t in range(ntile):
                                                par = tcount % 2
                                                tcount += 1
                                                xtt, t_x = xt[par], t_xt[par]
                                                dma("sp", xtt[:], src[tok0 + t * 128: tok0 + (t + 1) * 128, :], r=rsrc, w=[t_x])
                                                rstd = rms_rstd((junk, t_junk, ssn[par], t_ssn[par]), xtt[:], t_x, D, None)
                                                ts("dve", xtt[:], xtt[:], rstd, None, ALU.mult, r=[t_x, t_ssn[par]], w=[t_x])
                                                pA, tpA = ps[2 * par], tps[2 * par]
                                                pB, tpB = ps[2 * par + 1], tps[2 * par + 1]
                                                for k in range(8):
                                                    pp, tpp = (pA, tpA) if k < 4 else (pB, tpB)
                                                    tr(pp[:, (k % 4) * 128:(k % 4 + 1) * 128], xtt[:, k * 128:(k + 1) * 128], ident_f[:],
                                                       r=[t_x, t_const], w=[tpp])
                                                for k in range(8):
                                                    pp, tpp = (pA, tpA) if k < 4 else (pB, tpB)
                                                    o = hTg[:, k, t * 128:(t + 1) * 128]
                                                    i_ = pp[:, (k % 4) * 128:(k % 4 + 1) * 128]
                                                    if k % 2 == 0:
                                                        act(o, i_, AF.Identity, r=[tpp, t_lp], w=[t_hTg], bias=AB[:, 1, mv, k:k + 1],
                                                            scale=AB[:, 0, mv, k:k + 1])
                                                    else:
                                                        ts("dve", o, i_, AB[:, 0, mv, k:k + 1], AB[:, 1, mv, k:k + 1], ALU.mult, ALU.add,
                                                           r=[tpp, t_lp], w=[t_hTg])

                                            def proj(col0, M, tag=None):
                                                pb, tpb = ring.next()
                                                for k in range(8):
                                                    mm(pb[0:M, 0:N], win[:, k, col0:col0 + M], hTg[:, k, 0:N], k == 0, k == 7,
                                                       r=[t_win, t_hTg], w=[tpb])
                                                return pb, tpb

                                            if pas == 0:
                                                if not kv_only:
                                                    for c in range(2):
                                                        pb, tpb = proj(c * 128, 128)
                                                        act(uT[g % 2][:, c, 0:N], pb[:, 0:N], AF.Gelu_apprx_tanh, r=[tpb], w=[t_uT[g % 2][c]])
                                                for which in ((0, 1) if not kv_only else (1,)):
                                                    for h in range(4):
                                                        pb, tpb = proj(512 + which * 256 + h * 64, 64)
                                                        a0, a1, a2, a3 = [(tmpi + j) % 8 for j in range(4)]
                                                        tmpi += 4
                                                        qf, sqb, t1, t2 = tmpf[a0], tmpf[a1], tmpf[a2], tmpf[a3]
                                                        tqf, tsq, tt1, tt2 = t_tmpf[a0], t_tmpf[a1], t_tmpf[a2], t_tmpf[a3]
                                                        act(qf[0:64, 0:N], pb[0:64, 0:N], AF.Identity, r=[tpb], w=[tqf])
                                                        act(sqb[0:64, 0:N], pb[0:64, 0:N], AF.Square, r=[tpb], w=[tsq])
                                                        p2, tp2 = ring.next()
                                                        mm(p2[0:64, 0:N], blk64[:], sqb[0:64, 0:N], True, True, r=[t_const, tsq], w=[tp2])
                                                        act(sqb[0:64, 0:N], p2[0:64, 0:N], AF.Sqrt, r=[tp2], w=[tsq], bias=EPS, scale=1.0 / 32.0)
                                                        rcp(sqb[0:64, 0:N], sqb[0:64, 0:N], r=[tsq], w=[tsq])
                                                        stt(qf[0:64, 0:N], qf[0:64, 0:N], gqk[:, which:which + 1], sqb[0:64, 0:N], ALU.mult, ALU.mult,
                                                            r=[tqf, tsq, t_mp], w=[tqf])
                                                        if which == 0:
                                                            dst = qT[:, h, tok0:tok0 + N]
                                                            tdst = t_q[h][g]
                                                        else:
                                                            dst = kT[:, h, koff + tok0: koff + tok0 + N]
                                                            tdst = t_k[h][4 if isctx else g]
                                                        if not isctx:
                                                            p3, tp3 = ring.next()
                                                            mm(p3[0:64, 0:N], perm64[:], qf[0:64, 0:N], True, True, r=[t_const, tqf], w=[tp3])
                                                            tt("dve", t2[0:64, 0:N], p3[0:64, 0:N], ropes[:, tok0:tok0 + N], ALU.mult, r=[tp3, t_mp], w=[tt2])
                                                            tt("pool", t1[0:64, 0:N], qf[0:64, 0:N], ropec[:, tok0:tok0 + N], ALU.mult, r=[tqf, t_mp], w=[tt1])
                                                            tt("pool", dst, t1[0:64, 0:N], t2[0:64, 0:N], ALU.add, r=[tt1, tt2], w=[tdst])
                                                        else:
                                                            cp("pool", dst, qf[0:64, 0:N], r=[tqf], w=[tdst])
                                                for t in range(ntile):
                                                    pb, tpb = ring.next()
                                                    if not kv_only:
                                                        for k in range(8):
                                                            mm(pb[:, 0:256], hTg[:, k, t * 128:(t + 1) * 128], win[:, k, 256:512], k == 0, k == 7,
                                                               r=[t_win, t_hTg], w=[tpb])
                                                    for k in range(8):
                                                        mm(pb[:, 256:512], hTg[:, k, t * 128:(t + 1) * 128], win[:, k, 1024:1280], k == 0, k == 7,
                                                           r=[t_win, t_hTg], w=[tpb])
                                                    kti = ktile0 + g * 4 + t
                                                    cp("act", Vaug[:, kti, :, 0:64], pb[:, 256:512].rearrange("p (h e) -> p h e", h=4),
                                                       r=[tpb], w=[t_v[kti]])
                                                    if kv_only:
                                                        continue
                                                    vp = (g * 4 + t) % 2
                                                    a0 = tmpi % 8
                                                    tmpi += 1
                                                    vg, tvg = tmpf[a0], t_tmpf[a0]
                                                    act(vg[:, 0:256], pb[:, 0:256], AF.Gelu_apprx_tanh, r=[tpb], w=[tvg])
                                                    for h in range(4):
                                                        act(junk[:, h * 64:(h + 1) * 64], vg[:, h * 64:(h + 1) * 64], AF.Square, r=[tvg],
                                                            w=[t_junk, t_ssv[vp]], accum=ssv[vp][:, h:h + 1])
                                                    act(ssv[vp][:, 4:8], ssv[vp][:, 0:4], AF.Sqrt, r=[t_ssv[vp]], w=[t_ssv[vp]], bias=EPS, scale=1.0 / 64.0)
                                                    rcp(ssv[vp][:, 8:12], ssv[vp][:, 4:8], r=[t_ssv[vp]], w=[t_ssv[vp]])
                                                    for h in range(4):
                                                        stt(vn[vp][:, h * 64:(h + 1) * 64], vg[:, h * 64:(h + 1) * 64], ssv[vp][:, 8 + h:9 + h],
                                                            vngb[:, h * 64:(h + 1) * 64], ALU.mult, ALU.mult, r=[tvg, t_ssv[vp], t_mp], w=[t_vn[vp]])
                                                    for c in range(2):
                                                        pm_, tpm = ring.next()
                                                        for hh in range(2):
                                                            mm(pm_[:, hh * 128:(hh + 1) * 128], vn[vp][:, c * 128:(c + 1) * 128], wsT[:, 2 * c + hh, :], True, True,
                                                               r=[t_vn[vp], t_mp], w=[tpm])
                                                        a1 = tmpi % 8
                                                        tmpi += 1
                                                        mt, tmt = tmpf[a1], t_tmpf[a1]
                                                        for hh in range(2):
                                                            rows = slice(hh * 64, (hh + 1) * 64)
                                                            tt("dve", mt[rows, 0:128], pm_[rows, hh * 128:(hh + 1) * 128],
                                                               bsb[rows, (2 * c + hh) * 128:(2 * c + hh + 1) * 128], ALU.add, r=[tpm, t_mp], w=[tmt])
                                                        tt("pool", yT[:, c, tok0 + t * 128: tok0 + (t + 1) * 128], mt[:, 0:128],
                                                           uT[g % 2][:, c, t * 128:(t + 1) * 128], ALU.mult, r=[tmt, t_uT[g % 2][c]], w=[t_y[c][g]])
                                            else:
                                                for c in range(2):
                                                    pca, tpca = proj(0 + c * 128, 128)
                                                    pcg, tpcg = proj(256 + c * 128, 128)
                                                    a0 = tmpi % 8
                                                    tmpi += 1
                                                    sg, tsg = tmpf[a0], t_tmpf[a0]
                                                    act(sg[:, 0:N], pcg[:, 0:N], AF.Sigmoid, r=[tpcg], w=[tsg])
                                                    tt("dve", zbuf[:, c, 15 + tok0:15 + tok0 + N], pca[:, 0:N], sg[:, 0:N], ALU.mult, r=[tpca, tsg], w=[t_z[c][g]])
                                                for c in range(2):
                                                    pdb, tpdb = proj(512 + c * 128, 128)
                                                    cp("act", yT[:, 6 + c, tok0:tok0 + N], pdb[:, 0:N], r=[tpdb], w=[t_y[6 + c][g]])
                                                    pdc, tpdc = proj(768 + c * 128, 128)
                                                    pdh, tpdh = proj(1024 + c * 128, 128)
                                                    a0 = tmpi % 8
                                                    tmpi += 1
                                                    th, tth = tmpf[a0], t_tmpf[a0]
                                                    cp("act", th[:, 0:N], pdh[:, 0:N], r=[tpdh], w=[tth])
                                                    tt("dve", ebuf[:, c, 1 + tok0:1 + tok0 + N], pdc[:, 0:N], th[:, 0:N], ALU.mult, r=[tpdc, tth], w=[t_e[c][g]])
                                        P.barrier()
                                    if stop == f"p1{pas}":
                                        return
                                if kv_only:
                                    continue
                                with ExitStack() as p2s:
                                    Eb = [sb(p2s, f"Eb{i}", [128, 512], BF16) for i in range(4)]
                                    t_Eb = [T() for _ in range(4)]
                                    ybuf = [sb(p2s, f"ybuf{i}", [128, 4, 256], F32) for i in range(2)]
                                    t_yb = [T(), T()]
                                    ssb = [sb(p2s, f"ssb{i}", [128, 4, 12], F32) for i in range(2)]
                                    t_ssb = [T(), T()]
                                    rr = [sb(p2s, f"rr{i}", [128, 4], F32) for i in range(4)]
                                    t_rr = [T() for _ in range(4)]
                                    tq = [sb(p2s, f"tq{i}", [128, 64], F32) for i in range(4)]
                                    t_tq = [T() for _ in range(4)]
                                    ybn = [sb(p2s, f"ybn{i}", [128, 256], F32) for i in range(2)]
                                    t_ybn = [T(), T()]
                                    junk2 = sb(p2s, "junk2", [128, 512], BF16)
                                    t_junk2 = T()
                                    cf = [sb(p2s, f"cf{i}", [128, 512], F32) for i in range(8)]
                                    t_cf = [T() for _ in range(8)]
                                    ktiles = [16, 17] if isctx else list(range(18))
                                    sring = Ring([2, 3, 4, 5])
                                    oT = [[sb(p2s, f"oT{i}{m}", [65, 512], F32) for m in range(2)] for i in range(2)]
                                    t_oT = [[T(), T()], [T(), T()]]
                                    ei = 0
                                    it = 0
                                    ri = 0
                                    for g in range(ngr):
                                        tok0 = g * 512
                                        yb_, tyb = ybuf[g % 2], t_yb[g % 2]
                                        sb_, tsb = ssb[g % 2], t_ssb[g % 2]
                                        for h in range(4):
                                            pv = [(ps[m], tps[m]) for m in range(2)]
                                            oT_, toT_ = oT[it % 2], t_oT[it % 2]
                                            it += 1
                                            its = [(ki, kt, m) for ki, kt in enumerate(ktiles) for m in range(2)]

                                            def emit_score(ix):
                                                ki, kt, m = its[ix]
                                                kg = 4 if kt >= 16 else kt // 4
                                                psS, tpS = sring.next()
                                                mm(psS[:, 0:N], kT[m * 32:(m + 1) * 32, h, kt * 128:(kt + 1) * 128],
                                                   qT[m * 32:(m + 1) * 32, h, tok0:tok0 + N], True, True, r=[t_k[h][kg], t_q[h][g]], w=[tpS])
                                                return psS, tpS

                                            issued = []
                                            nxi = 0
                                            for ix, (ki, kt, m) in enumerate(its):
                                                while nxi < len(its) and nxi <= ix + 3:
                                                    issued.append(emit_score(nxi))
                                                    nxi += 1
                                                psS, tpS = issued.pop(0)
                                                E, tE = Eb[ei % 4], t_Eb[ei % 4]
                                                ei += 1
                                                act(E[:, 0:N], psS[:, 0:N], AF.Exp, r=[tpS], w=[tE])
                                                mm(pv[m][0][0:65, 0:N], Vaug[:, kt, h, :], E[:, 0:N], ki == 0, ki == len(ktiles) - 1,
                                                   r=[tE, t_v[kt]], w=[pv[m][1]])
                                            if bp_hit():
                                                P.barrier()
                                                return
                                            for m in range(2):
                                                cp("act" if m == 0 else "dve", oT_[m][:, 0:N], pv[m][0][0:65, 0:N], r=[pv[m][1]], w=[toT_[m]])
                                            if bp_hit():
                                                P.barrier()
                                                return
                                            for qt in range(ntile):
                                                for m in range(2):
                                                    tr(ps[6 + m][:, qt * 65:(qt + 1) * 65], oT_[m][0:65, qt * 128:(qt + 1) * 128], ident_f[0:65, 0:65],
                                                       r=[toT_[m], t_const], w=[tps[6 + m]])
                                            if bp_hit():
                                                P.barrier()
                                                return
                                            for qt in range(ntile):
                                                r_, tr_ = rr[ri % 4], t_rr[ri % 4]
                                                tq_, ttq = tq[ri % 4], t_tq[ri % 4]
                                                ri += 1
                                                rcp(r_[:, 0:1], ps[6][:, qt * 65 + 64: qt * 65 + 65], r=[tps[6]], w=[tr_])
                                                rcp(r_[:, 1:2], ps[7][:, qt * 65 + 64: qt * 65 + 65], r=[tps[7]], w=[tr_])
                                                tt("dve", r_[:, 2:3], r_[:, 1:2], lams[:, 4:5], ALU.mult, r=[tr_, t_mp], w=[tr_])
                                                ts("dve", tq_[:], ps[6][:, qt * 65: qt * 65 + 64], r_[:, 0:1], None, ALU.mult, r=[tps[6], tr_], w=[ttq])
                                                stt(yb_[:, qt, h * 64:(h + 1) * 64], ps[7][:, qt * 65: qt * 65 + 64], r_[:, 2:3], tq_[:], ALU.mult, ALU.add,
                                                    r=[tps[7], tr_, ttq], w=[tyb])
                                                act(junk2[:, 0:64], yb_[:, qt, h * 64:(h + 1) * 64], AF.Square, r=[tyb], w=[t_junk2, tsb],
                                                    accum=sb_[:, qt, h:h + 1])
                                            if bp_hit():
                                                P.barrier()
                                                return
                                        for qt in range(ntile):
                                            act(sb_[:, qt, 4:8], sb_[:, qt, 0:4], AF.Sqrt, r=[tsb], w=[tsb], bias=EPS, scale=1.0 / 64.0)
                                            rcp(sb_[:, qt, 8:12], sb_[:, qt, 4:8], r=[tsb], w=[tsb])
                                            if bp_hit():
                                                P.barrier()
                                                return
                                            yn, tyn = ybn[qt % 2], t_ybn[qt % 2]
                                            for h in range(4):
                                                stt(yn[:, h * 64:(h + 1) * 64], yb_[:, qt, h * 64:(h + 1) * 64], sb_[:, qt, 8 + h:9 + h], sublnb[:],
                                                    ALU.mult, ALU.mult, r=[tyb, tsb, t_mp], w=[tyn])
                                            if bp_hit():
                                                P.barrier()
                                                return
                                            for cc in range(2):
                                                tr(ps[6][:, cc * 128:(cc + 1) * 128], yn[:, cc * 128:(cc + 1) * 128], ident_f[:], r=[tyn, t_const], w=[tps[6]])
                                            if bp_hit():
                                                P.barrier()
                                                return
                                            for cc in range(2):
                                                cp("act", yT[:, 2 + cc, tok0 + qt * 128: tok0 + (qt + 1) * 128], ps[6][:, cc * 128:(cc + 1) * 128],
                                                   r=[tps[6]], w=[t_y[2 + cc][g]])
                                                if bp_hit():
                                                    P.barrier()
                                                    return
                                    if stop == "p2a":
                                        P.barrier()
                                        return
                                    cring = Ring([4, 5, 6, 7])
                                    ci = 0
                                    for g in range(ngr):
                                        tok0 = g * 512
                                        zc, tzc, zq, tzq = [], [], [], []
                                        for c in range(2):
                                            pcv, tpcv = cring.next()
                                            for k in range(31):
                                                mm(pcv[:, 0:N], diag[:, c, k, :], zbuf[:, c, tok0 + k: tok0 + k + N], k == 0, k == 30,
                                                   r=[t_diag] + t_z[c], w=[tpcv])
                                            a, b2 = cf[ci % 8], cf[(ci + 1) % 8]
                                            ta, tb2 = t_cf[ci % 8], t_cf[(ci + 1) % 8]
                                            ci += 2
                                            act(a[:, 0:N], pcv[:, 0:N], AF.Identity, r=[tpcv, t_mp], w=[ta], bias=pcol[:, c, 31:32])
                                            act(b2[:, 0:N], a[:, 0:N], AF.Square, r=[ta], w=[tb2])
                                            zc.append(a); tzc.append(ta); zq.append(b2); tzq.append(tb2)
                                        pme, tpme = cring.next()
                                        pms, tpms = cring.next()
                                        for c in range(2):
                                            mm(pme[:, 0:N], ones256[:], zc[c][:, 0:N], c == 0, c == 1, r=[t_ones, tzc[c]], w=[tpme])
                                        for c in range(2):
                                            mm(pms[:, 0:N], ones256[:], zq[c][:, 0:N], c == 0, c == 1, r=[t_ones, tzq[c]], w=[tpms])
                                        m2, tm2 = cf[ci % 8], t_cf[ci % 8]
                                        ci += 1
                                        act(m2[:, 0:N], pme[:, 0:N], AF.Square, r=[tpme], w=[tm2])
                                        tt("dve", m2[:, 0:N], pms[:, 0:N], m2[:, 0:N], ALU.subtract, r=[tpms, tm2], w=[tm2])
                                        ts("dve", m2[:, 0:N], m2[:, 0:N], 0.0, None, ALU.max, r=[tm2], w=[tm2])
                                        act(m2[:, 0:N], m2[:, 0:N], AF.Sqrt, r=[tm2], w=[tm2], bias=EPS, scale=1.0)
                                        rcp(m2[:, 0:N], m2[:, 0:N], r=[tm2], w=[tm2])
                                        for c in range(2):
                                            tt("dve", zc[c][:, 0:N], zc[c][:, 0:N], pme[:, 0:N], ALU.subtract, r=[tzc[c], tpme], w=[tzc[c]])
                                            tt("pool", zc[c][:, 0:N], zc[c][:, 0:N], m2[:, 0:N], ALU.mult, r=[tzc[c], tm2], w=[tzc[c]])
                                            act(yT[:, 4 + c, tok0:tok0 + N], zc[c][:, 0:N], AF.Silu, r=[tzc[c], t_mp], w=[t_y[4 + c][g]],
                                                bias=pcol[:, c, 33:34], scale=pcol[:, c, 32:33])
                                    if stop == "p2b":
                                        P.barrier()
                                        return
                                    for g in range(ngr):
                                        tok0 = g * 512
                                        for c in range(2):
                                            a, ta = cf[ci % 8], t_cf[ci % 8]
                                            ci += 1
                                            ts("dve", a[:, 0:N], ebuf[:, c, tok0:tok0 + N], pcol[:, c, 34:35], None, ALU.mult, r=t_e[c] + [t_mp], w=[ta])
                                            stt(a[:, 0:N], ebuf[:, c, tok0 + 1:tok0 + 1 + N], pcol[:, c, 35:36], a[:, 0:N], ALU.mult, ALU.add, r=t_e[c] + [ta], w=[ta])
                                            stt(a[:, 0:N], ebuf[:, c, tok0 + 2:tok0 + 2 + N], pcol[:, c, 36:37], a[:, 0:N], ALU.mult, ALU.add, r=t_e[c] + [ta], w=[ta])
                                            tt("pool", yT[:, 6 + c, tok0:tok0 + N], yT[:, 6 + c, tok0:tok0 + N], a[:, 0:N], ALU.mult, r=[t_y[6 + c][g], ta],
                                               w=[t_y[6 + c][g]])
                                    P.barrier()
                                    if stop == "p2":
                                        return
                                with ExitStack() as p3s:
                                    wout = sb(p3s, "wout", [128, 8, D], BF16)
                                    t_wout = T()
                                    dma("pool", wout[:], prm["w_out"][l].rearrange("(k p) f -> p k f", p=128), w=[t_wout])
                                    g1b = sb(p3s, "g1b", [128, D], F32)
                                    t_g1b = T()
                                    dma("sp", g1b[:], mod_d[l, mv:mv + 1, 2 * D:3 * D].partition_broadcast(128), r=[t_mod], w=[t_g1b])
                                    xt3 = [sb(p3s, f"xt3{i}", [128, D], F32) for i in range(2)]
                                    t_xt3 = [T(), T()]
                                    xo = [sb(p3s, f"xo{i}", [128, D], F32) for i in range(2)]
                                    t_xo = [T(), T()]
                                    for ti in range(n // 128):
                                        g = ti // 4
                                        par = ti % 2
                                        dma("sp", xt3[par][:], src[ti * 128:(ti + 1) * 128, :], r=rsrc, w=[t_xt3[par]])
                                        for dh in range(2):
                                            pb, tpb = ps[par * 2 + dh], tps[par * 2 + dh]
                                            for j in range(8):
                                                mm(pb[:, :], yT[:, j, ti * 128:(ti + 1) * 128], wout[:, j, dh * 512:(dh + 1) * 512], j == 0, j == 7,
                                                   r=[t_y[j][g], t_wout], w=[tpb])
                                            tt("dve", xo[par][:, dh * 512:(dh + 1) * 512], pb[:, :], g1b[:, dh * 512:(dh + 1) * 512], ALU.mult,
                                               r=[tpb, t_g1b], w=[t_xo[par]])
                                        tt("pool", xo[par][:], xo[par][:], xt3[par][:], ALU.add, r=[t_xo[par], t_xt3[par]], w=[t_xo[par]])
                                        dma("sp", xa_d[sq_["off"] + ti * 128: sq_["off"] + (ti + 1) * 128, :], xo[par][:], r=[t_xo[par]], w=[t_xa])
                                    P.barrier()
                                    if stop == "p3":
                                        dma("sp", dbg_d["dbg_xa"], xa_d, r=[t_xa], w=[T("dbg")])
                                        return
                    P.barrier()

                if dbg and "dbg_xa" in dbg_d and l == 0:
                    dma("sp", dbg_d["dbg_xa"], xa_d, r=[t_xa], w=[T("dbg")])
                    dma("sp", dbg_d["dbg_mod"], mod_d[0], r=[t_mod], w=[T("dbg2")])
                if skip_moe:
                    P.barrier()
                    continue
                if MOE_SPARSE:
                    tl_all = []
                    for s in seqs:
                        if s["ctx"] and not last:
                            for ti in range(s["n"] // 128):
                                r0 = s["off"] + ti * 128
                                tl_all.append((r0, xb_d[r0:r0 + 128, :], t_xb, 2))
                    for s in seqs:
                        if s["ctx"]:
                            continue
                        for ti in range(s["n"] // 128):
                            r0 = s["off"] + ti * 128
                            if last:
                                tl_all.append((r0, out_d[s["b"], ti * 128:(ti + 1) * 128, :], t_out, s["mv"]))
                            else:
                                tl_all.append((r0, xb_d[r0:r0 + 128, :], t_xb, s["mv"]))
                    NTT = len(tl_all)
                    NT = (NTT * 128 * 4) // 512 + NEXP
                    t_h2d, t_xp, t_yd = T("h2d"), T("xp"), T("yd")
                    with ExitStack() as me:
                        rw = sb(me, "rw", [128, 8, NEXP], F32)
                        rbb = sb(me, "rbb", [128, NEXP], F32)
                        bdn = sb(me, "bdn", [NEXP, D], F32)
                        bgu_b = sb(me, "bgub", [NEXP, 2 * D], BF16)
                        ltri = sb(me, "ltri", [128, 128], F32)
                        onesf = sb(me, "onesf", [128, 128], F32)
                        eiota = sb(me, "eiota", [128, 1], F32)
                        pk = sb(me, "pk", [128, 8], F32)
                        gates_all = sb(me, "gates_all", [128, NTT, NEXP], F32)
                        sidx = sb(me, "sidx", [128, NTT, 4], U32)
                        gk = sb(me, "gk", [128, NTT, 4], F32)
                        idxu = sb(me, "idxu", [128, NT, 8], U32)
                        oh = sb(me, "oh", [NEXP, NT], F32)
                        t_me, t_ga, t_sidx, t_gk, t_idx, t_oh = T(), T(), T(), T(), T(), T()
                        dma("sp", rw[:], prm["router_w"][l].rearrange("(k p) e -> p k e", p=128), w=[t_me])
                        dma("sp", rbb[:], prm["router_b"][l:l + 1, :].partition_broadcast(128), w=[t_me])
                        dma("sp", bdn[:], prm["exp_b_dn"][l], w=[t_me])
                        dma("pool", bgu_b[:], prm["exp_b_gu"][l], w=[t_me])
                        dma("sp", ltri[:], ltri_in, w=[t_me])
                        dma("sp", eiota[:], eiota_in, w=[t_me])
                        dma("sp", pk[:], pk_in[:, l * 8:(l + 1) * 8], w=[t_me])
                        mset("pool", onesf[:], 1.0, w=[t_me])
                        with ExitStack() as sa:
                            mask_all = sb(sa, "mask_all", [128, NTT, NEXP], F32)
                            rank_all = sb(sa, "rank_all", [128, NTT, NEXP], F32)
                            Ssum = sb(sa, "Ssum", [128, NEXP], F32)
                            t_mk, t_rk, t_S = T(), T(), T()
                            mset("dve", Ssum[:], 0.0, w=[t_S])
                            xm = [sb(sa, f"xm{i}", [128, D], F32) for i in range(2)]
                            t_xm = [T(), T()]
                            h2t = [sb(sa, f"h2t{i}", [128, D], F32) for i in range(2)]
                            t_h2t = [T(), T()]
                            h2f = [sb(sa, f"h2f{i}", [128, 8, 128], F32) for i in range(2)]
                            t_h2f = [T(), T()]
                            junkm = sb(sa, "junkm", [128, D], BF16)
                            t_junkm = T()
                            ssm = [sb(sa, f"ssm{i}", [128, 4], F32) for i in range(2)]
                            t_ssm = [T(), T()]
                            rt = [sb(sa, f"rt{i}", [128, 4, NEXP], F32) for i in range(2)]
                            t_rt = [T(), T()]
                            rs8 = [sb(sa, f"rs8{i}", [128, 12], F32) for i in range(2)]
                            A2b = sb(sa, "A2b", [128, D], F32)
                            B2b = sb(sa, "B2b", [128, D], F32)
                            n2gb = sb(sa, "n2gb", [128, D], F32)
                            t_ab = T()
                            dma("sp", n2gb[:], prm["norm2_g"][l:l + 1, :].partition_broadcast(128), w=[t_ab])
                            cur_mv_a = [None]

                            def front_a(ti):
                                r0, dst, tdst, mv = tl_all[ti]
                                par = ti % 2
                                if mv != cur_mv_a[0]:
                                    cur_mv_a[0] = mv
                                    dma("sp", A2b[:], mod_d[l, mv:mv + 1, 4 * D:5 * D].partition_broadcast(128), r=[t_mod], w=[t_ab])
                                    dma("sp", B2b[:], mod_d[l, mv:mv + 1, 3 * D:4 * D].partition_broadcast(128), r=[t_mod], w=[t_ab])
                                    stt(A2b[:], A2b[:], 1.0, n2gb[:], ALU.add, ALU.mult, r=[t_ab], w=[t_ab])
                                xtt, t_x = xm[par], t_xm[par]
                                dma("sp", xtt[:], xa_d[r0:r0 + 128, :], r=[t_xa], w=[t_x])
                                rstd = rms_rstd((junkm, t_junkm, ssm[par], t_ssm[par]), xtt[:], t_x, D, None)
                                ht, tht = h2t[par], t_h2t[par]
                                stt(ht[:], xtt[:], rstd, A2b[:], ALU.mult, ALU.mult, r=[t_x, t_ssm[par], t_ab], w=[tht])
                                tt("pool", ht[:], ht[:], B2b[:], ALU.add, r=[tht, t_ab], w=[tht])
                                dma("sp", h2_d[ti * 128:(ti + 1) * 128, :], ht[:], r=[tht], w=[t_h2d])
                                pA, tpA = ps[2 * par], tps[2 * par]
                                pB, tpB = ps[2 * par + 1], tps[2 * par + 1]
                                for k in range(8):
                                    pp, tpp = (pA, tpA) if k < 4 else (pB, tpB)
                                    tr(pp[:, (k % 4) * 128:(k % 4 + 1) * 128], ht[:, k * 128:(k + 1) * 128], ident_f[:], r=[tht, t_const], w=[tpp])
                                hf_, thf = h2f[par], t_h2f[par]
                                cp("act", hf_[:, 0:4, :], pA[:, :].rearrange("p (k t) -> p k t", k=4), r=[tpA], w=[thf])
                                cp("dve", hf_[:, 4:8, :], pB[:, :].rearrange("p (k t) -> p k t", k=4), r=[tpB], w=[thf])
                                pr, tpr = ps[4 + par], tps[4 + par]
                                for k in range(8):
                                    mm(pr[:, 0:NEXP], hf_[:, k, :], rw[:, k, :], k == 0, k == 7, r=[thf, t_me], w=[tpr])

                            def tail_a(ti):
                                par = ti % 2
                                pr, tpr = ps[4 + par], tps[4 + par]
                                R_, tR = rt[par], t_rt[par]
                                s8 = rs8[par]
                                tt("dve", R_[:, 0, :], pr[:, 0:NEXP], rbb[:], ALU.add, r=[tpr, t_me], w=[tR])
                                P.op("dve", (lambda o, i_: (lambda e: e.max(out=o, in_=i_)))(s8[:, 0:8], R_[:, 0, :]), [tR], [tR])
                                ts("dve", mask_all[:, ti, :], R_[:, 0, :], s8[:, 3:4], None, ALU.is_ge, r=[tR], w=[t_mk])
                                ts("dve", s8[:, 8:9], s8[:, 0:1], -1.0, None, ALU.mult, r=[tR], w=[tR])
                                act(R_[:, 2, :], R_[:, 0, :], AF.Exp, r=[tR], w=[tR], bias=s8[:, 8:9], scale=1.0)
                                tt("dve", R_[:, 2, :], R_[:, 2, :], mask_all[:, ti, :], ALU.mult, r=[tR, t_mk], w=[tR])
                                act(R_[:, 3, :], R_[:, 2, :], AF.Identity, r=[tR], w=[tR], accum=s8[:, 9:10])
                                rcp(s8[:, 10:11], s8[:, 9:10], r=[tR], w=[tR])
                                ts("dve", gates_all[:, ti, :], R_[:, 2, :], s8[:, 10:11], None, ALU.mult, r=[tR], w=[t_ga])
                                pk_, tpk = ps[6 + par], tps[6 + par]
                                mm(pk_[:, 0:NEXP], ltri[:], mask_all[:, ti, :], True, False, r=[t_me, t_mk], w=[tpk])
                                mm(pk_[:, 0:NEXP], onesf[:], Ssum[:], False, True, r=[t_me, t_S], w=[tpk])
                                cp("act", rank_all[:, ti, :], pk_[:, 0:NEXP], r=[tpk], w=[t_rk])
                                tt("dve", Ssum[:], Ssum[:], mask_all[:, ti, :], ALU.add, r=[t_S, t_mk], w=[t_S])

                            front_a(0)
                            for ti in range(NTT):
                                if ti + 1 < NTT:
                                    front_a(ti + 1)
                                tail_a(ti)

                            sm = sb(sa, "sm", [128, 8, NEXP], F32)
                            t_sm = T()
                            mm(ps[0][:, 0:NEXP], onesf[:], Ssum[:], True, True, r=[t_me, t_S], w=[tps[0]])
                            cp("dve", sm[:, 0, :], ps[0][:, 0:NEXP], r=[tps[0]], w=[t_sm])
                            ts("dve", sm[:, 1, :], sm[:, 0, :], 0.0, None, ALU.is_gt, r=[t_sm], w=[t_sm])
                            for i in range(1, 10):
                                stt(sm[:, 1, :], sm[:, 0, :], 512.0 * i, sm[:, 1, :], ALU.is_gt, ALU.add, r=[t_sm], w=[t_sm])
                            cp("dve", sm[:, 2, :], sm[:, 1, :], r=[t_sm], w=[t_sm])
                            src_i = 2
                            for sh in (1, 2, 4, 8, 16):
                                dst_i = 5 - src_i
                                cp("dve", sm[:, dst_i, 0:sh], sm[:, src_i, 0:sh], r=[t_sm], w=[t_sm])
                                tt("dve", sm[:, dst_i, sh:NEXP], sm[:, src_i, sh:NEXP], sm[:, src_i, 0:NEXP - sh], ALU.add, r=[t_sm], w=[t_sm])
                                src_i = dst_i
                            incl = sm[:, src_i, :]
                            tt("dve", sm[:, 4, :], incl, sm[:, 1, :], ALU.subtract, r=[t_sm], w=[t_sm])
                            ts("dve", sm[:, 4, :], sm[:, 4, :], 512.0, 1.0, ALU.mult, ALU.add, r=[t_sm], w=[t_sm])
                            ejb = sb(sa, "ejb", [128, NT], F32)
                            idxf = sb(sa, "idxf", [128, NT, 8], F32)
                            t_ej = T()
                            for j in range(NT):
                                P.op("dve", (lambda o, i_, thr, acc_: (lambda e: e.tensor_scalar(o, i_, thr, 0.0, ALU.is_lt, ALU.add, accum_out=acc_)))(
                                    sm[:, 5, :], incl, j + 0.5, ejb[:, j:j + 1]), [t_sm], [t_sm, t_ej])
                            ts("dve", ejb[:], ejb[:], float(NEXP - 1), None, ALU.min, r=[t_ej], w=[t_ej])
                            ts("dve", oh[:], ejb[0:NEXP, :], eiota[0:NEXP, 0:1], None, ALU.is_equal, r=[t_ej, t_me], w=[t_oh])
                            for k in range(8):
                                ts("dve", idxf[:, :, k], ejb[:], 1024.0, pk[:, k:k + 1], ALU.mult, ALU.add, r=[t_ej, t_me], w=[t_ej])
                            cp("dve", idxu[:], idxf[:], r=[t_ej], w=[t_idx])
                            hb_ = [sb(sa, f"hsc{i}", [128, D], F32) for i in range(4)]
                            t_hb = [T() for _ in range(4)]
                            NSET = 4
                            vals = [sb(sa, f"val{i}", [128, NEXP], F32) for i in range(NSET)]
                            t8s = [sb(sa, f"t8{i}", [128, 8], F32) for i in range(NSET)]
                            eqbs = [sb(sa, f"eqb{i}", [128, NEXP], F32) for i in range(NSET)]
                            sifs = [sb(sa, f"sif{i}", [128, 4], F32) for i in range(NSET)]
                            t_vals = [T() for _ in range(NSET)]

                            def slot_seq(ti, si):
                                val, t8, eqb, sif, t_val = vals[si], t8s[si], eqbs[si], sifs[si], t_vals[si]
                                tt("dve", val[:], rank_all[:, ti, :], sm[:, 4, :], ALU.add, r=[t_rk, t_sm], w=[t_val])
                                yield
                                tt("dve", val[:], val[:], mask_all[:, ti, :], ALU.mult, r=[t_val, t_mk], w=[t_val])
                                yield
                                P.op("dve", (lambda o, i_: (lambda e: e.max(out=o, in_=i_)))(t8[:], val[:]), [t_val], [t_val])
                                yield
                                ts("dve", sif[:], t8[:, 0:4], -1.0, None, ALU.add, r=[t_val], w=[t_val])
                                yield
                                cp("dve", sidx[:, ti, :], sif[:], r=[t_val], w=[t_sidx])
                                yield
                                b_, tb_ = hb_[ti % 4], t_hb[ti % 4]
                                dma("sp", b_[:], h2_d[ti * 128:(ti + 1) * 128, :], r=[t_h2d], w=[tb_])
                                for k in range(4):
                                    P.dma("pool", (lambda o, i_: (lambda e: e.indirect_dma_start(out=xp_d, out_offset=IndirectOffsetOnAxis(ap=o, axis=0), in_=i_, in_offset=None)))(
                                        sidx[:, ti, k:k + 1], b_[:]), [tb_, t_sidx], [t_xp])
                                for k in range(4):
                                    stt(eqb[:], val[:], t8[:, k:k + 1], gates_all[:, ti, :], ALU.is_equal, ALU.mult, r=[t_val, t_ga], w=[t_val])
                                    yield
                                    P.op("dve", (lambda o, i_: (lambda e: e.reduce_sum(o, i_, axis=mybir.AxisListType.X)))(gk[:, ti, k:k + 1], eqb[:]),
                                         [t_val], [t_gk])
                                    yield

                            for base in range(0, NTT, NSET):
                                gens = [slot_seq(ti, ti - base) for ti in range(base, min(NTT, base + NSET))]
                                while gens:
                                    alive = []
                                    for gq in gens:
                                        try:
                                            next(gq)
                                            alive.append(gq)
                                        except StopIteration:
                                            pass
                                    gens = alive
                            P.barrier()
                        with ExitStack() as sd:
                            wgu = [sb(sd, f"wgu{i}", [128, 8, 2 * D], BF16) for i in range(2)]
                            t_wgu = [T(), T()]
                            wdn = sb(sd, "wdn", [128, 8, D], BF16)
                            t_wdn = T()
                            xt_ = [sb(sd, f"xts{i}", [128, D], F32) for i in range(2)]
                            t_xt_ = [T(), T()]
                            h2T = [sb(sd, f"h2T{i}", [128, 8, 512], BF16) for i in range(2)]
                            t_h2T = [T(), T()]
                            hid = [sb(sd, f"hid{i}", [128, 8, 512], BF16) for i in range(2)]
                            t_hid = [[T() for _ in range(8)] for _ in range(2)]
                            sw = [[sb(sd, f"sw{i}{j}", [128, 512], F32) for j in range(3)] for i in range(2)]
                            t_sw = [[T() for _ in range(3)] for _ in range(2)]
                            yo = [sb(sd, f"yo{i}", [128, D], F32) for i in range(2)]
                            t_yo = [T(), T()]
                            ones32 = sb(sd, "ones32", [NEXP, 512], BF16)
                            ohr = [sb(sd, f"ohr{i}", [NEXP, 512], BF16) for i in range(2)]
                            t_ohr = [T(), T()]
                            t_o32 = T()
                            mset("pool", ones32[:], 1.0, w=[t_o32])
                            gring = Ring([2, 3, 4, 5])
                            dring = Ring([6, 7])
                            sub = 0
                            stg = [sb(sd, f"stg{i}", [128, 2 * D], F32) for i in range(4)]
                            t_stg = [T() for _ in range(4)]
                            t_wguk = [[T() for _ in range(8)] for _ in range(2)]
                            t_wdnk = [T() for _ in range(8)]
                            sctr = [0]
                            pending = []

                            def issue_gather(flat, ix, dst_ap, t_dst, ncols, eng):
                                i = sctr[0] % 4
                                sctr[0] += 1
                                P.dma("pool", (lambda o, ixx: (lambda e: e.indirect_dma_start(out=o, out_offset=None, in_=flat, in_offset=IndirectOffsetOnAxis(ap=ixx, axis=0))))(
                                    stg[i][:, 0:ncols], ix), [t_idx], [t_stg[i]])
                                pending.append((i, dst_ap, t_dst, ncols, eng))

                            def flush_casts():
                                while pending:
                                    i, dst_ap, t_dst, ncols, eng = pending.pop(0)
                                    cp(eng, dst_ap, stg[i][:, 0:ncols], r=[t_stg[i]], w=[t_dst])

                            for k in range(8):
                                issue_gather(wgu_flat, idxu[:, 0, k:k + 1], wgu[0][:, k, :], t_wguk[0][k], 2 * D, "act")
                                flush_casts()
                            subc = [0]

                            def emit_tr(jx):
                                hTx, thTx = h2T[jx % 2], t_h2T[jx % 2]
                                for q_ in range(4):
                                    par = subc[0] % 2
                                    subc[0] += 1
                                    dma("sp", xt_[par][:], xp_d[jx * 512 + q_ * 128: jx * 512 + (q_ + 1) * 128, :], r=[t_xp], w=[t_xt_[par]])
                                    for k in range(8):
                                        pp, tpp = (ps[0], tps[0]) if k < 4 else (ps[1], tps[1])
                                        tr(pp[:, (k % 4) * 128:(k % 4 + 1) * 128], xt_[par][:, k * 128:(k + 1) * 128], ident_f[:], r=[t_xt_[par], t_const], w=[tpp])
                                    cp("act", hTx[:, 0:4, q_ * 128:(q_ + 1) * 128], ps[0][:, :].rearrange("p (k t) -> p k t", k=4), r=[tps[0]], w=[thTx])
                                    cp("dve", hTx[:, 4:8, q_ * 128:(q_ + 1) * 128], ps[1][:, :].rearrange("p (k t) -> p k t", k=4), r=[tps[1]], w=[thTx])
                            for j in range(NT):
                                wb, twbk = wgu[j % 2], t_wguk[j % 2]
                                hT_, thT = h2T[j % 2], t_h2T[j % 2]
                                ts("dve", ohr[j % 2][:], ones32[:], oh[:, j:j + 1], None, ALU.mult, r=[t_o32, t_oh], w=[t_ohr[j % 2]])
                                if j == 0:
                                    emit_tr(0)
                                hb, thb = hid[j % 2], t_hid[j % 2]
                                for jj in range(8):
                                    flush_casts()
                                    issue_gather(wdn_flat, idxu[:, j, jj:jj + 1], wdn[:, jj, :], t_wdnk[jj], D, "dve")
                                    if j + 1 < NT:
                                        issue_gather(wgu_flat, idxu[:, j + 1, jj:jj + 1], wgu[(j + 1) % 2][:, jj, :], t_wguk[(j + 1) % 2][jj], 2 * D, "act")
                                    pgl, tpgl = gring.next()
                                    pup, tpup = gring.next()
                                    for k in range(8):
                                        mm(pgl[:, :], wb[:, k, jj * 128:(jj + 1) * 128], hT_[:, k, :], k == 0, False, r=[twbk[k], thT], w=[tpgl])
                                    mm(pgl[:, :], bgu_b[:, jj * 128:(jj + 1) * 128], ohr[j % 2][:], False, True, r=[t_me, t_ohr[j % 2]], w=[tpgl])
                                    for k in range(8):
                                        mm(pup[:, :], wb[:, k, D + jj * 128: D + (jj + 1) * 128], hT_[:, k, :], k == 0, False, r=[twbk[k], thT], w=[tpup])
                                    mm(pup[:, :], bgu_b[:, D + jj * 128: D + (jj + 1) * 128], ohr[j % 2][:], False, True, r=[t_me, t_ohr[j % 2]], w=[tpup])
                                    sp_ = jj % 2
                                    a_, s_, u_ = sw[sp_]
                                    ta_, ts_, tu_ = t_sw[sp_]
                                    ts("dve", a_[:], pgl[:, :], 7.0, None, ALU.min, r=[tpgl], w=[ta_])
                                    act(s_[:], a_[:], AF.Sigmoid, r=[ta_], w=[ts_], scale=1.702)
                                    ts("dve", u_[:], pup[:, :], 7.0, -7.0, ALU.min, ALU.max, r=[tpup], w=[tu_])
                                    tt("dve", a_[:], a_[:], s_[:], ALU.mult, r=[ta_, ts_], w=[ta_])
                                    stt(hb[:, jj, :], u_[:], 1.0, a_[:], ALU.add, ALU.mult, r=[ta_, tu_], w=[thb[jj]])
                                flush_casts()
                                if j + 1 < NT:
                                    emit_tr(j + 1)
                                for q_ in range(4):
                                    yy, tyy = yo[q_ % 2], t_yo[q_ % 2]
                                    for dh in range(2):
                                        pd, tpd = dring.next()
                                        for jj in range(8):
                                            mm(pd[:, :], hb[:, jj, q_ * 128:(q_ + 1) * 128], wdn[:, jj, dh * 512:(dh + 1) * 512], jj == 0, jj == 7,
                                               r=[thb[jj], t_wdnk[jj]], w=[tpd])
                                        cp("act" if dh == 0 else "dve", yy[:, dh * 512:(dh + 1) * 512], pd[:, :], r=[tpd], w=[tyy])
                                    dma("sp", y_d[j * 512 + q_ * 128: j * 512 + (q_ + 1) * 128, :], yy[:], r=[tyy], w=[t_yd])
                            P.barrier()
                        with ExitStack() as se:
                            xm = [sb(se, f"xme{i}", [128, D], F32) for i in range(2)]
                            t_xm = [T(), T()]
                            acc = [sb(se, f"acce{i}", [128, D], F32) for i in range(2)]
                            t_acc = [T(), T()]
                            yk = [sb(se, f"yk{i}", [128, D], F32) for i in range(4)]
                            t_yk = [T() for _ in range(4)]
                            gT = [sb(se, f"gTe{i}", [NEXP, 128], F32) for i in range(2)]
                            t_gT = [T(), T()]
                            g2b = sb(se, "g2be", [128, D], F32)
                            t_g2b = T()
                            cur_mv = None
                            yi = 0
                            for ti, (r0, dst, tdst, mv) in enumerate(tl_all):
                                par = ti % 2
                                if mv != cur_mv:
                                    cur_mv = mv
                                    dma("sp", g2b[:], mod_d[l, mv:mv + 1, 5 * D:6 * D].partition_broadcast(128), r=[t_mod], w=[t_g2b])
                                dma("sp", xm[par][:], xa_d[r0:r0 + 128, :], r=[t_xa], w=[t_xm[par]])
                                pg, tpg = ps[4 + par], tps[4 + par]
                                tr(pg[0:NEXP, 0:128], gates_all[:, ti, :], ident_f[:], r=[t_ga, t_const], w=[tpg])
                                cp("act", gT[par][:], pg[0:NEXP, 0:128], r=[tpg], w=[t_gT[par]])
                                for dh in range(2):
                                    pb, tpb = ps[par * 2 + dh], tps[par * 2 + dh]
                                    mm(pb[:, :], gT[par][:], bdn[:, dh * 512:(dh + 1) * 512], True, True, r=[t_gT[par], t_me], w=[tpb])
                                    cp("act", acc[par][:, dh * 512:(dh + 1) * 512], pb[:, :], r=[tpb], w=[t_acc[par]])
                                for k in range(4):
                                    y_, ty_ = yk[yi % 4], t_yk[yi % 4]
                                    yi += 1
                                    P.dma("pool", (lambda o, ix: (lambda e: e.indirect_dma_start(out=o, out_offset=None, in_=y_d, in_offset=IndirectOffsetOnAxis(ap=ix, axis=0))))(
                                        y_[:], sidx[:, ti, k:k + 1]), [t_sidx, t_yd], [ty_])
                                    stt(acc[par][:], y_[:], gk[:, ti, k:k + 1], acc[par][:], ALU.mult, ALU.add, r=[ty_, t_gk, t_acc[par]], w=[t_acc[par]])
                                tt("dve", acc[par][:], acc[par][:], g2b[:], ALU.mult, r=[t_acc[par], t_g2b], w=[t_acc[par]])
                                tt("pool", xm[par][:], xm[par][:], acc[par][:], ALU.add, r=[t_xm[par], t_acc[par]], w=[t_xm[par]])
                                dma("sp", dst, xm[par][:], r=[t_xm[par]], w=[tdst])
                            P.barrier()
                    if dbg and "dbg_xb" in dbg_d and l == 0:
                        dma("sp", dbg_d["dbg_xb"], xb_d, r=[t_xb], w=[T("dbg3")])
                    continue
                with ExitStack() as me:
                    rw = sb(me, f"rw{l}", [128, 8, NEXP], F32)
                    rbb = sb(me, f"rbb{l}", [128, NEXP], F32)
                    bdn = sb(me, f"bdn{l}", [NEXP, D], F32)
                    bgu_r = sb(me, f"bgur{l}", [NEXP, 2 * D], F32)
                    bguT = sb(me, f"bguT{l}", [128, 16, NEXP], F32)
                    t_me = T("moeparams")
                    dma("sp", rw[:], prm["router_w"][l].rearrange("(k p) e -> p k e", p=128), w=[t_me])
                    dma("sp", rbb[:], prm["router_b"][l:l + 1, :].partition_broadcast(128), w=[t_me])
                    dma("sp", bdn[:], prm["exp_b_dn"][l], w=[t_me])
                    dma("sp", bgu_r[:], prm["exp_b_gu"][l], w=[t_me])
                    for j in range(16):
                        tr(ps[j % 8][:, 0:NEXP], bgu_r[:, j * 128:(j + 1) * 128], ident_f[0:NEXP, 0:NEXP], r=[t_me, t_const], w=[tps[j % 8]])
                        cp("dve", bguT[:, j, :], ps[j % 8][:, 0:NEXP], r=[tps[j % 8]], w=[t_me])
                    wgu = [sb(me, f"wgu{l}{i}", [128, 8, 2 * D], BF16) for i in range(2)]
                    t_wgu = [T(), T()]
                    wdn = sb(me, f"wdn{l}", [128, 8, D], BF16)
                    t_wdn = T()
                    h2T = sb(me, f"h2T{l}", [128, 8, 1024], BF16)
                    t_h2 = [T() for _ in range(8)]
                    acc = sb(me, f"acc{l}", [128, 8, D], F32)
                    t_acc = [T() for _ in range(8)]
                    gates = sb(me, f"gates{l}", [128, 8, NEXP], F32)
                    t_gt = [T() for _ in range(8)]
                    hid = [sb(me, f"hid{l}{i}", [128, 8, 512], BF16) for i in range(2)]
                    t_hid = [[T() for _ in range(8)] for _ in range(2)]
                    sw = [[sb(me, f"sw{l}{i}{j}", [128, 512], F32) for j in range(3)] for i in range(2)]
                    t_sw = [[T() for _ in range(3)] for _ in range(2)]
                    xm = [sb(me, f"xm{l}{i}", [128, D], F32) for i in range(2)]
                    t_xm = [T(), T()]
                    h2f = [sb(me, f"h2f{l}{i}", [128, 8, 128], F32) for i in range(2)]
                    t_h2f = [T(), T()]
                    junkm = sb(me, f"junkm{l}", [128, D], BF16)
                    t_junkm = T()
                    ssm = [sb(me, f"ssm{l}{i}", [128, 4], F32) for i in range(2)]
                    t_ssm = [T(), T()]
                    rt = [sb(me, f"rt{l}{i}", [128, 4, NEXP], F32) for i in range(2)]
                    t_rt = [T(), T()]
                    rs8 = [sb(me, f"rs8{l}{i}", [128, 12], F32) for i in range(2)]
                    gT = [sb(me, f"gT{l}{i}", [NEXP, 128], F32) for i in range(2)]
                    t_gT = [T(), T()]
                    g2b = sb(me, f"g2b{l}", [128, D], F32)
                    t_g2b = T()

                    groups = []
                    if not last:
                        tl = []
                        for s in seqs:
                            if s["ctx"]:
                                for ti in range(s["n"] // 128):
                                    r0 = s["off"] + ti * 128
                                    tl.append((r0, xb_d[r0:r0 + 128, :], t_xb))
                        groups.append((2, tl))
                    for s in seqs:
                        if s["ctx"]:
                            continue
                        for hf in range(2):
                            tl = []
                            for ti in range(8):
                                r0 = s["off"] + hf * 1024 + ti * 128
                                if last:
                                    dst = out_d[s["b"], hf * 1024 + ti * 128: hf * 1024 + (ti + 1) * 128, :]
                                    tl.append((r0, dst, t_out))
                                else:
                                    tl.append((r0, xb_d[r0:r0 + 128, :], t_xb))
                            groups.append((s["mv"], tl))

                    ecount = 0
                    tcount = 0
                    for (mv, tl) in groups:
                        ntl = len(tl)
                        nsub = ntl // 4
                        dma("sp", g2b[:], mod_d[l, mv:mv + 1, 5 * D:6 * D].partition_broadcast(128), r=[t_mod], w=[t_g2b])
                        for ti, (r0, dst, tdst) in enumerate(tl):
                            par = tcount % 2
                            tcount += 1
                            xtt, t_x = xm[par], t_xm[par]
                            dma("sp", xtt[:], xa_d[r0:r0 + 128, :], r=[t_xa], w=[t_x])
                            rstd = rms_rstd((junkm, t_junkm, ssm[par], t_ssm[par]), xtt[:], t_x, D, None)
                            ts("dve", xtt[:], xtt[:], rstd, None, ALU.mult, r=[t_x, t_ssm[par]], w=[t_x])
                            pA, tpA = ps[2 * par], tps[2 * par]
                            pB, tpB = ps[2 * par + 1], tps[2 * par + 1]
                            for k in range(8):
                                pp, tpp = (pA, tpA) if k < 4 else (pB, tpB)
                                tr(pp[:, (k % 4) * 128:(k % 4 + 1) * 128], xtt[:, k * 128:(k + 1) * 128], ident_f[:], r=[t_x, t_const], w=[tpp])
                            hf_, thf = h2f[par], t_h2f[par]
                            for k in range(8):
                                pp, tpp = (pA, tpA) if k < 4 else (pB, tpB)
                                i_ = pp[:, (k % 4) * 128:(k % 4 + 1) * 128]
                                if k % 2 == 0:
                                    act(hf_[:, k, :], i_, AF.Identity, r=[tpp, t_lp], w=[thf], bias=AB[:, 3, mv, k:k + 1], scale=AB[:, 2, mv, k:k + 1])
                                else:
                                    ts("dve", hf_[:, k, :], i_, AB[:, 2, mv, k:k + 1], AB[:, 3, mv, k:k + 1], ALU.mult, ALU.add, r=[tpp, t_lp], w=[thf])
                            cp("pool", h2T[:, :, ti * 128:(ti + 1) * 128], hf_[:, :, :], r=[thf], w=[t_h2[ti]])
                            pr, tpr = ps[4 + par], tps[4 + par]
                            for k in range(8):
                                mm(pr[:, 0:NEXP], hf_[:, k, :], rw[:, k, :], k == 0, k == 7, r=[thf, t_me], w=[tpr])
                            R_, tR = rt[par], t_rt[par]
                            s8 = rs8[par]
                            tt("dve", R_[:, 0, :], pr[:, 0:NEXP], rbb[:], ALU.add, r=[tpr, t_me], w=[tR])
                            P.op("dve", (lambda o, i_: (lambda e: e.max(out=o, in_=i_)))(s8[:, 0:8], R_[:, 0, :]), [tR], [tR])
                            ts("dve", R_[:, 1, :], R_[:, 0, :], s8[:, 3:4], None, ALU.is_ge, r=[tR], w=[tR])
                            ts("dve", s8[:, 8:9], s8[:, 0:1], -1.0, None, ALU.mult, r=[tR], w=[tR])
                            act(R_[:, 2, :], R_[:, 0, :], AF.Exp, r=[tR], w=[tR], bias=s8[:, 8:9], scale=1.0)
                            tt("dve", R_[:, 2, :], R_[:, 2, :], R_[:, 1, :], ALU.mult, r=[tR], w=[tR])
                            act(R_[:, 3, :], R_[:, 2, :], AF.Identity, r=[tR], w=[tR], accum=s8[:, 9:10])
                            rcp(s8[:, 10:11], s8[:, 9:10], r=[tR], w=[tR])
                            ts("dve", gates[:, ti, :], R_[:, 2, :], s8[:, 10:11], None, ALU.mult, r=[tR], w=[t_gt[ti]])
                            pg, tpg = ps[6], tps[6]
                            tr(pg[0:NEXP, 0:128], gates[:, ti, :], ident_f[:], r=[t_gt[ti], t_const], w=[tpg])
                            cp("act", gT[par][:], pg[0:NEXP, 0:128], r=[tpg], w=[t_gT[par]])
                            for dh in range(2):
                                pb, tpb = ps[7], tps[7]
                                mm(pb[:, :], gT[par][:], bdn[:, dh * 512:(dh + 1) * 512], True, True, r=[t_gT[par], t_me], w=[tpb])
                                cp("act", acc[:, ti, dh * 512:(dh + 1) * 512], pb[:, :], r=[tpb], w=[t_acc[ti]])
                        gring = Ring([0, 1, 2, 3])
                        dring = Ring([4, 5, 6, 7])
                        for ex in range(NEXP):
                            wb, twb = wgu[ecount % 2], t_wgu[ecount % 2]
                            ecount += 1
                            dma("pool", wdn[:], prm["exp_w_dn"][l, ex].rearrange("(k p) f -> p k f", p=128), w=[t_wdn])
                            dma("pool", wb[:], prm["exp_w_gu"][l, ex].rearrange("(k p) f -> p k f", p=128), w=[twb])
                            for sgi in range(nsub):
                                hb, thb = hid[sgi % 2], t_hid[sgi % 2]
                                rh = [t_h2[sgi * 4 + q_] for q_ in range(4)]
                                for j in range(8):
                                    pgl, tpgl = gring.next()
                                    pup, tpup = gring.next()
                                    for k in range(8):
                                        mm(pgl[:, :], wb[:, k, j * 128:(j + 1) * 128], h2T[:, k, sgi * 512:(sgi + 1) * 512], k == 0, k == 7,
                                           r=[twb] + rh, w=[tpgl])
                                    for k in range(8):
                                        mm(pup[:, :], wb[:, k, D + j * 128: D + (j + 1) * 128], h2T[:, k, sgi * 512:(sgi + 1) * 512], k == 0, k == 7,
                                           r=[twb] + rh, w=[tpup])
                                    sp_ = j % 2
                                    a_, s_, u_ = sw[sp_]
                                    ta_, ts_, tu_ = t_sw[sp_]
                                    ts("dve", a_[:], pgl[:, :], bguT[:, j, ex:ex + 1], 7.0, ALU.add, ALU.min, r=[tpgl, t_me], w=[ta_])
                                    act(s_[:], a_[:], AF.Sigmoid, r=[ta_], w=[ts_], scale=1.702)
                                    act(u_[:], pup[:, :], AF.Identity, r=[tpup, t_me], w=[tu_], bias=bguT[:, 8 + j, ex:ex + 1])
                                    ts("dve", u_[:], u_[:], 7.0, -7.0, ALU.min, ALU.max, r=[tu_], w=[tu_])
                                    tt("pool", a_[:], a_[:], s_[:], ALU.mult, r=[ta_, ts_], w=[ta_])
                                    stt(hb[:, j, :], u_[:], 1.0, a_[:], ALU.add, ALU.mult, r=[ta_, tu_], w=[thb[j]])
                                for q_ in range(4):
                                    ti = sgi * 4 + q_
                                    for dh in range(2):
                                        pd, tpd = dring.next()
                                        for j in range(8):
                                            mm(pd[:, :], hb[:, j, q_ * 128:(q_ + 1) * 128], wdn[:, j, dh * 512:(dh + 1) * 512], j == 0, j == 7,
                                               r=[thb[j], t_wdn], w=[tpd])
                                        stt(acc[:, ti, dh * 512:(dh + 1) * 512], pd[:, :], gates[:, ti, ex:ex + 1], acc[:, ti, dh * 512:(dh + 1) * 512],
                                            ALU.mult, ALU.add, r=[tpd, t_gt[ti], t_acc[ti]], w=[t_acc[ti]])
                        for ti, (r0, dst, tdst) in enumerate(tl):
                            par = tcount % 2
                            tcount += 1
                            xtt, t_x = xm[par], t_xm[par]
                            dma("sp", xtt[:], xa_d[r0:r0 + 128, :], r=[t_xa], w=[t_x])
                            tt("dve", acc[:, ti, :], acc[:, ti, :], g2b[:], ALU.mult, r=[t_acc[ti], t_g2b], w=[t_acc[ti]])
                            tt("pool", xtt[:], xtt[:], acc[:, ti, :], ALU.add, r=[t_x, t_acc[ti]], w=[t_x])
                            dma("sp", dst, xtt[:], r=[t_x], w=[tdst])
                    P.barrier()
                if dbg and "dbg_xb" in dbg_d and l == 0:
                    dma("sp", dbg_d["dbg_xb"], xb_d, r=[t_xb], w=[T("dbg3")])
        try:
            _layers()
        except _Stop:
            pass
        P.barrier()
        P.emit()
    return nc


def _consts():
    ident = np.eye(128, dtype=np.float32)
    blk = np.zeros((64, 64), np.float32)
    blk[0:32, 0:32] = 1.0
    blk[32:64, 32:64] = 1.0
    perm = np.zeros((64, 64), np.float32)
    for i in range(64):
        d = i % 32
        base = i - d
        half = (d // 16) * 16
        dd = d % 16
        partner = base + half + ((dd + 8) % 16)
        perm[partner, i] = 1.0
    rows = SEQ // 64
    row = np.repeat(np.arange(rows, dtype=np.float32), 64)
    col = np.tile(np.arange(64, dtype=np.float32), rows)
    nf = 8
    inv = (np.float32(10000.0) ** (-np.arange(nf, dtype=np.float32) / np.float32(nf))).astype(np.float32)
    ar = (row[:, None] * inv).astype(np.float32)
    ac = (col[:, None] * inv).astype(np.float32)
    C = np.zeros((64, SEQ), np.float32)
    S = np.zeros((64, SEQ), np.float32)
    for i in range(64):
        d = i % 32
        ang = ar if d < 16 else ac
        dd = d % 16
        f = dd % 8
        C[i] = np.cos(ang[:, f])
        S[i] = (-np.sin(ang[:, f])) if dd < 8 else np.sin(ang[:, f])
    ltri = np.triu(np.ones((128, 128), np.float32), k=1)
    eiota = np.arange(128, dtype=np.float32).reshape(128, 1)
    pk = np.zeros((128, 16), np.float32)
    for l in range(DEPTH):
        for k in range(8):
            pk[:, l * 8 + k] = l * NEXP * 1024 + k * 128 + np.arange(128)
    return dict(k_ident=ident, k_blk=blk, k_perm=perm, k_ropec=C, k_ropes=S, k_ltri=ltri, k_eiota=eiota, k_pk=pk)


_NC_CACHE = {}


def _make_in_maps(inputs):
    f32 = lambda a: np.ascontiguousarray(np.asarray(a, dtype=np.float32))
    consts = _consts()
    shared = {}
    for k, shp in PARAM_SHAPES.items():
        shared[k] = f32(inputs[k]).reshape(shp)
    x = f32(inputs["x"]); ctx = f32(inputs["ctx"]); c = f32(inputs["c"]); c_ctx = f32(inputs["c_ctx"]).reshape(1, D)
    in_maps = []
    for i in range(NCORES):
        m = dict(shared)
        m.update(consts)
        m["x"] = x[i * NB:(i + 1) * NB]
        m["ctx"] = ctx[i * NB:(i + 1) * NB]
        m["c"] = np.ascontiguousarray(np.concatenate([c[i * NB:(i + 1) * NB], c_ctx], axis=0))
        in_maps.append(m)
    return in_maps


def kernel(**inputs):
    if "nc" not in _NC_CACHE:
        _NC_CACHE["nc"] = build_nc()
    nc = _NC_CACHE["nc"]
    in_maps = _make_in_maps(inputs)
    res = run_bass_kernel_spmd(nc, in_maps, core_ids=list(range(NCORES)))
    out = np.concatenate([np.asarray(r["out"], dtype=np.float32) for r in res.results], axis=0)
    return out
```
